# Optimizing a Trainium2 kernel written in Bass

```python
import math
import jax, jax.numpy as jnp
from jax import lax
import numpy as np

D_MODEL = 1024
BATCH = 1
SEQ = 16384
DEPTH = 2

N_RET_HEADS = 4
RET_QK_DIM = D_MODEL // N_RET_HEADS
RET_V_DIM = 2 * RET_QK_DIM
RET_QK_TOTAL = N_RET_HEADS * RET_QK_DIM
RET_V_TOTAL = N_RET_HEADS * RET_V_DIM
RET_IN_WIDTH = 2 * RET_QK_TOTAL + 2 * RET_V_TOTAL
RET_CHUNK = 128
ROPE_BASE = 10000.0
POOL_WINDOWS = (2, 4, 8, 16)
POOL_GROUPS = len(POOL_WINDOWS)
POOL_GROUP_WIDTH = D_MODEL // POOL_GROUPS
D_FF = ((8 * D_MODEL // 3 + 255) // 256) * 256
N_EXPERTS = 8
TOP_K = 2
D_FF_EXPERT = 7 * D_MODEL // 2
MOE_BLOCK = 256
N_MOD = 6
EPS = 1e-6
N_A = (DEPTH + 1) // 2
N_B = DEPTH // 2

kernel_name = 'hybrid_retention_pool_moe_adaln'


def _rmsnorm(x, gain):
    xf = x.astype(jnp.float32)
    y = xf * lax.rsqrt(jnp.mean(xf * xf, axis=-1, keepdims=True) + EPS)
    return (y * gain.astype(jnp.float32)).astype(x.dtype)


def _modulate(h, shift, scale):
    return h * (1.0 + scale) + shift


def _rotary(t, pos):
    half = t.shape[-1] // 2
    inv = ROPE_BASE ** (-jnp.arange(half, dtype=jnp.float32) / half)
    ang = pos.astype(jnp.float32)[:, None] * inv[None, :]
    cos = jnp.cos(ang)[None, :, None, :]
    sin = jnp.sin(ang)[None, :, None, :]
    t1, t2 = t[..., :half], t[..., half:]
    return jnp.concatenate([t1 * cos - t2 * sin, t1 * sin + t2 * cos], axis=-1)


def _retention(h, w_in, gn_gain, w_out):
    bsz, seq, _ = h.shape
    H, DK, DV, C = N_RET_HEADS, RET_QK_DIM, RET_V_DIM, RET_CHUNK
    n_chunks = seq // C
    proj = h @ w_in
    q, k, v, g = jnp.split(proj, [RET_QK_TOTAL, 2 * RET_QK_TOTAL, 2 * RET_QK_TOTAL + RET_V_TOTAL], axis=-1)
    pos = jnp.arange(seq)
    q = _rotary(q.reshape(bsz, seq, H, DK).astype(jnp.float32), pos)
    k = _rotary(k.reshape(bsz, seq, H, DK).astype(jnp.float32), pos) * (DK ** -0.5)
    v = v.reshape(bsz, seq, H, DV).astype(jnp.float32)

    def to_chunks(t):
        return t.reshape(bsz, n_chunks, C, H, t.shape[-1]).transpose(0, 3, 1, 2, 4)

    qc, kc, vc = to_chunks(q), to_chunks(k), to_chunks(v)
    log_gamma = jnp.log1p(-jnp.exp2(-5.0 - jnp.arange(H, dtype=jnp.float32)))
    idx = jnp.arange(C, dtype=jnp.float32)
    diff = idx[:, None] - idx[None, :]
    d_intra = jnp.where(diff >= 0, jnp.exp(log_gamma[:, None, None] * jnp.maximum(diff, 0.0)), 0.0)
    scores = jnp.einsum('bhncd,bhnmd->bhncm', qc, kc) * d_intra[None, :, None]
    intra = jnp.einsum('bhncm,bhnme->bhnce', scores, vc)
    q_dec = jnp.exp(log_gamma[:, None] * (idx + 1.0))
    k_dec = jnp.exp(log_gamma[:, None] * (C - 1.0 - idx))
    chunk_dec = jnp.exp(log_gamma * C)
    qs = (qc * q_dec[None, :, None, :, None]).transpose(2, 0, 1, 3, 4)
    ks = (kc * k_dec[None, :, None, :, None]).transpose(2, 0, 1, 3, 4)
    vs = vc.transpose(2, 0, 1, 3, 4)

    def step(state, inp):
        qn, kn, vn = inp
        out = jnp.einsum('bhcd,bhde->bhce', qn, state)
        state = state * chunk_dec[None, :, None, None] + jnp.einsum('bhcd,bhce->bhde', kn, vn)
        return state, out

    state0 = jnp.zeros((bsz, H, DK, DV), jnp.float32)
    _, cross = lax.scan(step, state0, (qs, ks, vs))
    o = intra + cross.transpose(1, 2, 0, 3, 4)
    o = o.transpose(0, 2, 3, 1, 4).reshape(bsz, seq, H, DV)
    mu = jnp.mean(o, axis=-1, keepdims=True)
    var = jnp.mean(jnp.square(o - mu), axis=-1, keepdims=True)
    o = ((o - mu) * lax.rsqrt(var + EPS)).reshape(bsz, seq, RET_V_TOTAL) * gn_gain.astype(jnp.float32)
    y = jax.nn.silu(g.astype(jnp.float32)) * o
    return y.astype(h.dtype) @ w_out


def _pool_mixer(h, w_pool, b_pool, scale):
    bsz, seq, dm = h.shape
    hf = h.astype(jnp.float32).reshape(bsz, seq, POOL_GROUPS, POOL_GROUP_WIDTH)
    t1 = jnp.arange(1, seq + 1, dtype=jnp.float32)
    outs = []
    for gi, w in enumerate(POOL_WINDOWS):
        xg = hf[:, :, gi]
        cs = jnp.cumsum(jnp.pad(xg, ((0, 0), (w, 0), (0, 0))), axis=1)
        win_sum = cs[:, w:] - cs[:, :seq]
        cnt = jnp.minimum(t1, float(w))
        outs.append(win_sum / cnt[None, :, None] - xg)
    pooled = jnp.stack(outs, axis=2)
    y = jnp.einsum('bsgc,gcd->bsgd', pooled, w_pool.astype(jnp.float32)) + b_pool.astype(jnp.float32)
    return (y.reshape(bsz, seq, dm) * scale.astype(jnp.float32)).astype(h.dtype)


def _swiglu(h, w_gate, w_up, w_down):
    return (jax.nn.silu(h @ w_gate) * (h @ w_up)) @ w_down


def _moe_swiglu(h, w_router, w_gate, w_up, w_down):
    bsz, seq, dm = h.shape
    T = bsz * seq
    xt = h.reshape(T, dm)
    logits = (xt @ w_router).astype(jnp.float32)
    top_logits, top_idx = lax.top_k(logits, TOP_K)
    gates = jax.nn.softmax(top_logits, axis=-1)
    e_flat = top_idx.reshape(-1)
    tok_flat = jnp.repeat(jnp.arange(T, dtype=jnp.int32), TOP_K)
    g_flat = gates.reshape(-1)
    order = jnp.argsort(e_flat)
    e_sorted, tok_sorted, g_sorted = e_flat[order], tok_flat[order], g_flat[order]
    counts = jnp.zeros((N_EXPERTS,), jnp.int32).at[e_flat].add(1)
    padded = ((counts + MOE_BLOCK - 1) // MOE_BLOCK) * MOE_BLOCK
    start_unp = jnp.cumsum(counts) - counts
    pad_end = jnp.cumsum(padded)
    start_pad = pad_end - padded
    n_assign = T * TOP_K
    rank = jnp.arange(n_assign, dtype=jnp.int32) - start_unp[e_sorted]
    dest = start_pad[e_sorted] + rank
    cap = ((n_assign + MOE_BLOCK - 1) // MOE_BLOCK) * MOE_BLOCK + N_EXPERTS * MOE_BLOCK
    n_blocks = cap // MOE_BLOCK
    buf_tok = jnp.full((cap,), T, jnp.int32).at[dest].set(tok_sorted)
    buf_gate = jnp.zeros((cap,), jnp.float32).at[dest].set(g_sorted)
    block_start = jnp.arange(n_blocks, dtype=jnp.int32) * MOE_BLOCK
    block_e = jnp.minimum(jnp.sum(block_start[:, None] >= pad_end[None, :], axis=1), N_EXPERTS - 1)
    x_pad = jnp.concatenate([xt, jnp.zeros((1, dm), xt.dtype)], axis=0)
    xb = x_pad[buf_tok].reshape(n_blocks, MOE_BLOCK, dm)

    def expert_block(args):
        xblk, e = args
        return _swiglu(xblk, w_gate[e], w_up[e], w_down[e])

    yb = lax.map(expert_block, (xb, block_e)).reshape(cap, dm)
    yb = (yb.astype(jnp.float32) * buf_gate[:, None]).astype(h.dtype)
    y = jnp.zeros((T + 1, dm), h.dtype).at[buf_tok].add(yb)
    return y[:T].reshape(bsz, seq, dm)


def setup_inputs(seed: int = 0) -> dict:
    key = jax.random.key(seed)
    ks = jax.random.split(key, 24)
    f32 = jnp.float32
    nrm = lambda k, shape, s: jax.random.normal(k, shape, f32) * s
    D = D_MODEL
    return {
        'x': nrm(ks[0], (BATCH, SEQ, D), 1.0),
        'c': nrm(ks[1], (BATCH, D), 1.0),
        'ada_w': nrm(ks[2], (DEPTH, D, N_MOD * D), 0.5 * D ** -0.5),
        'ada_b': nrm(ks[3], (DEPTH, N_MOD * D), 0.01),
        'norm_gain': 1.0 + nrm(ks[4], (DEPTH, 2, D), 0.05),
        'ret_w_in': nrm(ks[5], (N_A, D, RET_IN_WIDTH), D ** -0.5),
        'ret_gn_gain': 1.0 + nrm(ks[6], (N_A, RET_V_TOTAL), 0.05),
        'ret_w_out': nrm(ks[7], (N_A, RET_V_TOTAL, D), RET_V_TOTAL ** -0.5),
        'ffn_w_gate': nrm(ks[8], (N_A, D, D_FF), D ** -0.5),
        'ffn_w_up': nrm(ks[9], (N_A, D, D_FF), D ** -0.5),
        'ffn_w_down': nrm(ks[10], (N_A, D_FF, D), D_FF ** -0.5),
        'pool_w': nrm(ks[11], (N_B, POOL_GROUPS, POOL_GROUP_WIDTH, POOL_GROUP_WIDTH), POOL_GROUP_WIDTH ** -0.5),
        'pool_b': nrm(ks[12], (N_B, POOL_GROUPS, POOL_GROUP_WIDTH), 0.01),
        'pool_scale': 1.0 + nrm(ks[13], (N_B, D), 0.1),
        'moe_router': nrm(ks[14], (N_B, D, N_EXPERTS), D ** -0.5),
        'moe_w_gate': nrm(ks[15], (N_B, N_EXPERTS, D, D_FF_EXPERT), D ** -0.5),
        'moe_w_up': nrm(ks[16], (N_B, N_EXPERTS, D, D_FF_EXPERT), D ** -0.5),
        'moe_w_down': nrm(ks[17], (N_B, N_EXPERTS, D_FF_EXPERT, D), D_FF_EXPERT ** -0.5),
        'final_norm_gain': 1.0 + nrm(ks[18], (D,), 0.05),
    }


def reference(x, c, ada_w, ada_b, norm_gain, ret_w_in, ret_gn_gain, ret_w_out,
              ffn_w_gate, ffn_w_up, ffn_w_down, pool_w, pool_b, pool_scale,
              moe_router, moe_w_gate, moe_w_up, moe_w_down, final_norm_gain):
    c_act = jax.nn.silu(c)
    for i in range(DEPTH):
        j = i // 2
        mod = (c_act @ ada_w[i] + ada_b[i])[:, None, :]
        sh1, sc1, g1, sh2, sc2, g2 = jnp.split(mod, N_MOD, axis=-1)
        h = _modulate(_rmsnorm(x, norm_gain[i, 0]), sh1, sc1)
        if i % 2 == 0:
            m = _retention(h, ret_w_in[j], ret_gn_gain[j], ret_w_out[j])
        else:
            m = _pool_mixer(h, pool_w[j], pool_b[j], pool_scale[j])
        x = x + g1 * m
        h = _modulate(_rmsnorm(x, norm_gain[i, 1]), sh2, sc2)
        if i % 2 == 0:
            f = _swiglu(h, ffn_w_gate[j], ffn_w_up[j], ffn_w_down[j])
        else:
            f = _moe_swiglu(h, moe_router[j], moe_w_gate[j], moe_w_up[j], moe_w_down[j])
        x = x + g2 * f
    return _rmsnorm(x, final_norm_gain)
```

```python
import numpy as np
from contextlib import ExitStack
import concourse.bass as bass
import concourse.mybir as mybir
from concourse.bass_utils import run_bass_kernel_spmd

F32 = mybir.dt.float32
BF16 = mybir.dt.bfloat16
U32 = mybir.dt.uint32
AF = mybir.ActivationFunctionType
ALU = mybir.AluOpType
AX = mybir.AxisListType

NCORES = 8
D = 1024
TOWN = 2048
NDEEP = 32
NTILE = NDEEP + 1 + 16
HALO = NDEEP
DFF = 2816
NF = DFF // 128
DFE = 3584
NFE = DFE // 128
EPS = 1e-6
CAPS = [1024] * 8
OFFS = [sum(CAPS[:r]) for r in range(8)]
TOTS = sum(CAPS)
CAPMAX = max(CAPS)
NSLMAX = CAPMAX // 128
GAMMA = [1.0 - 2.0 ** (-5.0 - h) for h in range(4)]

SAME_ENGINE_SYNC = True
RET_ORDER = 0
NPJ = 4
SEQ_HEADS = True
RET_W2 = 4
RET_W3 = 2


class Buf:
    __slots__ = ("ap", "lw", "rd", "name")

    def __init__(self, ap=None, name=""):
        self.ap = ap
        self.lw = None
        self.rd = {}
        self.name = name


class Eng:
    def __init__(self, fw, name, obj, sem):
        self.fw = fw
        self.name = name
        self.obj = obj
        self.sem = sem
        self.count = 0
        self.waited = {}
        self.is_pe = name == "pe"

    def _wait(self, tok):
        if tok is None:
            return
        key, val = tok
        if key is self.sem and (self.is_pe or not SAME_ENGINE_SYNC):
            return
        if self.waited.get(id(key), 0) >= val:
            return
        self.waited[id(key)] = val
        self.obj.wait_ge(key, val)

    def deps(self, reads, writes):
        for b in reads:
            self._wait(b.lw)
        for b in writes:
            self._wait(b.lw)
            for k, v in list(b.rd.items()):
                self._wait((self.fw.semobj[k], v))

    def emit(self, fn, reads=(), writes=(), signal=True):
        self.deps(reads, writes)
        ins = fn(self.obj)
        if signal:
            ins.then_inc(self.sem, 1)
            self.count += 1
            tok = (self.sem, self.count)
        else:
            tok = (self.sem, self.count + 1)
        self.fw.note(tok, reads, writes)
        return tok


class DSem:
    def __init__(self, sem):
        self.sem = sem
        self.count = 0


class DSemPool:
    def __init__(self, fw, name, n):
        self.sems = [fw.dsem(f"{name}{i}") for i in range(n)]
        self.i = 0

    def next(self, q):
        d = self.sems[self.i % len(self.sems)]
        self.i += 1
        if d.count > 0:
            q._wait((d.sem, d.count))
        return d


class FW:
    def __init__(self, nc, stack):
        self.nc = nc
        self.stack = stack
        self.semobj = {}
        self.dsems = []
        self.pe = self._mk("pe", nc.tensor)
        self.act = self._mk("act", nc.scalar)
        self.dve = self._mk("dve", nc.vector)
        self.pool = self._mk("pool", nc.gpsimd)
        self.sp = self._mk("sp", nc.sync)
        self.engines = [self.pe, self.act, self.dve, self.pool, self.sp]

    def _sem(self, name):
        s = self.stack.enter_context(self.nc.semaphore(name))
        self.semobj[id(s)] = s
        return s

    def _mk(self, name, obj):
        return Eng(self, name, obj, self._sem("s_" + name))

    def dsem(self, name):
        d = DSem(self._sem("d_" + name))
        self.dsems.append(d)
        return d

    def note(self, tok, reads, writes):
        k = id(tok[0])
        for b in reads:
            if b.rd.get(k, 0) < tok[1]:
                b.rd[k] = tok[1]
        for b in writes:
            b.lw = tok
            b.rd = {}

    def dma(self, q, out, in_, dsem, reads=(), writes=(), **kw):
        if isinstance(dsem, DSemPool):
            dsem = dsem.next(q)
        q.deps(reads, writes)
        q.obj.dma_start(out=out, in_=in_, **kw).then_inc(dsem.sem, 16)
        dsem.count += 16
        tok = (dsem.sem, dsem.count)
        self.note(tok, reads, writes)
        return tok

    def sync_on(self, toks):
        for e in self.engines:
            for t in toks:
                if t is not None:
                    e._wait(t)

    def barrier(self):
        for e in self.engines:
            for o in self.engines:
                if o is not e and o.count > 0:
                    e._wait((o.sem, o.count))
            for d in self.dsems:
                if d.count > 0:
                    e._wait((d.sem, d.count))


def _host_tables(core):
    start = core * TOWN
    pos = start - (NDEEP + 1) * 128 + np.arange(NTILE * 128)
    inv = (10000.0 ** (-np.arange(128, dtype=np.float32) / 128)).astype(np.float32)
    ang = (np.maximum(pos, 0).astype(np.float32)[:, None] * inv[None, :]).astype(np.float32)
    cs = np.concatenate([np.cos(ang.astype(np.float64)), np.sin(ang.astype(np.float64))], -1).astype(np.float32)
    valid = np.zeros((128, NTILE), np.float32)
    for j in range(NTILE):
        valid[:, j] = 1.0 if pos[j * 128] >= 0 else 0.0
    wins = (2, 4, 8, 16)
    bc = np.zeros((128, 4, 128), np.float32)
    bp = np.zeros((128, 4, 128), np.float32)
    bc0 = np.zeros((128, 4, 128), np.float32)
    for g, w in enumerate(wins):
        for t in range(128):
            for tp in range(t - w + 1, t + 1):
                if tp >= 0:
                    bc[tp, g, t] += 1.0 / w
                else:
                    bp[128 + tp, g, t] += 1.0 / w
            bc[t, g, t] -= 1.0
            gt = start + t
            cnt = min(gt + 1, w)
            for tp in range(t - w + 1, t + 1):
                if tp >= 0:
                    bc0[tp, g, t] += 1.0 / cnt
            bc0[t, g, t] -= 1.0
    if core > 0:
        bp0 = bp.copy()
        bc0 = bc.copy()
    else:
        bp0 = np.zeros_like(bp)
    return dict(cs=cs, valid=valid, pbc=bc.reshape(128, 512), pbp=bp.reshape(128, 512), pbc0=bc0.reshape(128, 512), pbp0=bp0.reshape(128, 512))


def _const_tables():
    c = np.arange(128, dtype=np.float64)
    mask = np.zeros((128, 4, 128), np.float32)
    qdb = np.zeros((128, 1024), np.float32)
    kdb = np.zeros((128, 1024), np.float32)
    for h in range(4):
        lg = np.log1p(-2.0 ** (-5.0 - h))
        mm = np.exp(-lg * (c + 1.0))[:, None] * (c[None, :] >= c[:, None]) / 16.0
        mask[:, h, :] = mm
        qdb[:, h * 256:(h + 1) * 256] = np.exp(lg * (c + 1.0))[:, None]
        kdb[:, h * 256:(h + 1) * 256] = (np.exp(lg * (127.0 - c)) / 16.0)[:, None]
    mt = np.zeros((128, 96), np.float32)
    mt[:, 0:8] = np.asarray(CAPS, np.float32)[None, :]
    mt[:, 8:16] = np.asarray(OFFS, np.float32)[None, :]
    mt[:, 16:24] = np.arange(8, dtype=np.float32)[None, :]
    lt = (np.arange(8)[None, :] < np.arange(8)[:, None]).astype(np.float32)
    mt[:, 32:96] = lt.reshape(1, 64)
    return dict(rmask=mask.reshape(128, 512), qdb=qdb, kdb=kdb, moetab=mt)


def build_program(upto="all"):
    nc = bass.Bass("TRN2", target_bir_lowering=False)

    def din(name, shape, dt=F32):
        return nc.dram_tensor(name, list(shape), dt, kind="ExternalInput").ap()

    xext = din("xext", [NTILE * 128, D])
    cs_d = din("cs", [NTILE * 128, 256])
    valid_d = din("valid", [128, NTILE])
    rmask_d = din("rmask", [128, 512])
    qdb_d = din("qdb", [128, 1024])
    kdb_d = din("kdb", [128, 1024])
    pbc_d = din("pbc", [128, 512])
    pbp_d = din("pbp", [128, 512])
    pbc0_d = din("pbc0", [128, 512])
    pbp0_d = din("pbp0", [128, 512])
    moetab_d = din("moetab", [128, 96])
    c_d = din("c", [1, D])
    ada_w = din("ada_w", [2, D, 6 * D])
    ada_b = din("ada_b", [2, 6 * D])
    norm_gain = din("norm_gain", [2, 2, D])
    ret_w_in = din("ret_w_in", [1, D, 6144])
    ret_gn = din("ret_gn_gain", [1, 2048])
    ret_w_out = din("ret_w_out", [1, 2048, D])
    ffn_wg = din("ffn_w_gate", [1, D, DFF])
    ffn_wu = din("ffn_w_up", [1, D, DFF])
    ffn_wd = din("ffn_w_down", [1, DFF, D])
    pool_w = din("pool_w", [1, 4, 256, 256])
    pool_b = din("pool_b", [1, 4, 256])
    pool_scale = din("pool_scale", [1, D])
    moe_router = din("moe_router", [1, D, 8])
    moe_wg = din("moe_w_gate", [1, 8, D, DFE])
    moe_wu = din("moe_w_up", [1, 8, D, DFE])
    moe_wd = din("moe_w_down", [1, 8, DFE, D])
    fgain = din("final_norm_gain", [D])
    out_d = nc.dram_tensor("out", [TOWN, D], F32, kind="ExternalOutput").ap()

    with ExitStack() as top:
        fw = FW(nc, top)
        pe, act, dve, pool, sp = fw.pe, fw.act, fw.dve, fw.pool, fw.sp

        uid = [0]

        def sb(stack, name, shape, dt):
            uid[0] += 1
            return stack.enter_context(nc.sbuf_tensor(f"{name}_{uid[0]}", list(shape), dt))

        def ps(stack, name, shape, dt):
            uid[0] += 1
            return stack.enter_context(nc.psum_tensor(f"{name}_{uid[0]}", list(shape), dt))

        XRES = sb(top, "XRES", [128, 17, D], F32)
        xres_b = [Buf(name=f"xres{i}") for i in range(17)]
        ident = sb(top, "ident", [128, 128], BF16); ident_b = Buf()
        ones_row = sb(top, "ones_row", [1, 128], F32); ones_b = Buf()
        cb = sb(top, "cb", [128, 8, 128], F32); cb_b = Buf()
        eps_t = sb(top, "eps_t", [128, 1], F32)
        d_const = DSemPool(fw, "const", 8)
        d_x = [fw.dsem("x0"), fw.dsem("x1")]
        d_w = [fw.dsem(f"w{i}") for i in range(4)]
        d_out = DSemPool(fw, "out", 2)
        d_win = fw.dsem("win")
        d_pb = [fw.dsem(f"pb{i}") for i in range(4)]

        pool.emit(lambda e: e.memset(ident[:], 1.0), writes=[ident_b])
        pool.emit(lambda e: e.affine_select(ident[:], ident[:], [[-1, 128]], ALU.is_equal, 0.0, base=0, channel_multiplier=1),
                  reads=[ident_b], writes=[ident_b])
        pool.emit(lambda e: e.memset(ones_row[:], 1.0), writes=[ones_b])
        eps_b = Buf()
        pool.emit(lambda e: e.memset(eps_t[:], EPS), writes=[eps_b])
        HS = nc.dram_tensor("hs_scratch", [TOTS, D], BF16)
        YS = nc.dram_tensor("y_scratch", [TOTS, D], F32)
        hs_b = Buf()
        d_hs = fw.dsem("hs")
        if upto == "all":
            ZT = sb(top, "ZT", [128, D], BF16); zt_b = Buf()
            pool.emit(lambda e: e.memset(ZT[:], 0.0), writes=[zt_b])
            for n in range(TOTS // 128):
                fw.dma(pool, HS[n * 128:(n + 1) * 128, :], ZT[:], d_hs, reads=[zt_b], writes=[hs_b])
                hs_b.lw = None
            hs_b.lw = (d_hs.sem, d_hs.count)

        with ExitStack() as st:
            crow = sb(st, "crow", [1, D], F32); crow_b = Buf()
            pcb = ps(st, "pcb", [128, 8, 128], F32); pcb_b = Buf()
            fw.dma(sp, crow[:], c_d[:, :], d_const, writes=[crow_b])
            act.emit(lambda e: e.activation(crow[:], crow[:], AF.Silu), reads=[crow_b], writes=[crow_b])
            for kc in range(8):
                pe.emit(lambda e: e.matmul(pcb[:, kc, :], crow[:, kc * 128:(kc + 1) * 128], ones_row[:], start=True, stop=True),
                        reads=[crow_b, ones_b], writes=[pcb_b], signal=(kc == 7))
            dve.emit(lambda e: e.tensor_copy(cb[:], pcb[:]), reads=[pcb_b], writes=[cb_b])
            fw.barrier()

        def compute_mod(layer, half, MODB, modb_b):
            with ExitStack() as st:
                awt = [sb(st, f"awt{i}", [128, 8, 512], F32) for i in range(2)]
                awt_b = [Buf(), Buf()]
                brow = [sb(st, f"brow{i}", [1, 512], F32) for i in range(2)]
                brow_b = [Buf(), Buf()]
                gbc = sb(st, "gbc", [128, D], F32); gbc_b = Buf()
                pm = [ps(st, f"pmod{i}", [128, 512], F32) for i in range(2)]
                pm_b = [Buf(), Buf()]
                tok = fw.dma(sp, gbc[:], norm_gain[layer, half, :].partition_broadcast(128), d_const, writes=[gbc_b])
                for b in range(6):
                    col = half * 3072 + b * 512
                    i = b % 2
                    fw.dma(sp, awt[i][:], ada_w[layer, :, col:col + 512].rearrange("(k p) n -> p k n", p=128), d_w[i], writes=[awt_b[i]])
                    fw.dma(sp, brow[i][:], ada_b[layer:layer + 1, col:col + 512], d_w[2 + i], writes=[brow_b[i]])
                    for kc in range(8):
                        pe.emit(lambda e: e.matmul(pm[i][:], cb[:, kc, :], awt[i][:, kc, :], start=(kc == 0), stop=False),
                                reads=[cb_b, awt_b[i]], writes=[pm_b[i]], signal=False)
                    pe.emit(lambda e: e.matmul(pm[i][:], ones_row[:], brow[i][:], start=False, stop=True),
                            reads=[ones_b, brow_b[i]], writes=[pm_b[i]])
                    act.emit(lambda e: e.copy(MODB[:, b // 2, (b % 2) * 512:(b % 2) * 512 + 512], pm[i][:]), reads=[pm_b[i]], writes=[modb_b])
                dve.emit(lambda e: e.scalar_tensor_tensor(MODB[:, 1, :], MODB[:, 1, :], 1.0, gbc[:], ALU.add, ALU.mult),
                         reads=[modb_b, gbc_b], writes=[modb_b])
                fw.barrier()

        MODS = nc.dram_tensor("mods_scratch", [4, 3072], F32)
        mods_b = [[Buf(), Buf()] for _ in range(4)]
        d_mods = [fw.dsem("mods0"), fw.dsem("mods1")]

        def mod_rows_gen(stack, items):
            NA = 4
            awt = [sb(stack, f"bawt{i}", [128, 8, 512], F32) for i in range(NA)]; awt_b = [Buf() for _ in range(NA)]
            brow = [sb(stack, f"bbrow{i}", [1, 512], F32) for i in range(NA)]; brow_b = [Buf() for _ in range(NA)]
            rowo = [sb(stack, f"browo{i}", [1, 512], F32) for i in range(2)]; rowo_b = [Buf(), Buf()]
            pm = [ps(stack, f"bpmod{i}", [128, 512], F32) for i in range(2)]; pm_b = [Buf(), Buf()]
            d_bw = [fw.dsem(f"bw{i}") for i in range(NA)]
            blocks = [(layer, half, b) for (layer, half) in items for b in range(6)]

            def issue(n):
                layer, half, b = blocks[n]
                col = half * 3072 + b * 512
                a = n % NA
                fw.dma(sp, awt[a][:], ada_w[layer, :, col:col + 512].rearrange("(k p) n -> p k n", p=128), d_bw[a], writes=[awt_b[a]])
                awt_b[a].lw = None
                fw.dma(sp, brow[a][:], ada_b[layer:layer + 1, col:col + 512], d_bw[a], writes=[brow_b[a]])
                awt_b[a].lw = brow_b[a].lw

            for n in range(min(NA - 1, len(blocks))):
                issue(n)
            for n, (layer, half, b) in enumerate(blocks):
                idx = layer * 2 + half
                a = n % NA
                i = n % 2
                if n + NA - 1 < len(blocks):
                    issue(n + NA - 1)
                if True:
                    yield
                    for kc in range(8):
                        pe.emit(lambda e: e.matmul(pm[i][:], cb[:, kc, :], awt[a][:, kc, :], start=(kc == 0), stop=False),
                                reads=[cb_b, awt_b[a]], writes=[pm_b[i]], signal=False)
                    pe.emit(lambda e: e.matmul(pm[i][:], ones_row[:], brow[a][:], start=False, stop=True),
                            reads=[ones_b, brow_b[a]], writes=[pm_b[i]])
                    yield
                    act.emit(lambda e: e.copy(rowo[i][:], pm[i][0:1, :]), reads=[pm_b[i]], writes=[rowo_b[i]])
                    fw.dma(sp, MODS[idx:idx + 1, b * 512:(b + 1) * 512], rowo[i][:], d_mods[i], reads=[rowo_b[i]], writes=[mods_b[idx][i]])
                    yield

        def load_mod(layer, half, MODB, modb_b, stack):
            idx = layer * 2 + half
            gbc = sb(stack, "gbc", [128, D], F32); gbc_b = Buf()
            fw.dma(sp, gbc[:], norm_gain[layer, half, :].partition_broadcast(128), d_const, writes=[gbc_b])
            fw.dma(sp, MODB[:].rearrange("p s d -> p (s d)"), MODS[idx, :].partition_broadcast(128), d_const, reads=mods_b[idx], writes=[modb_b])
            dve.emit(lambda e: e.scalar_tensor_tensor(MODB[:, 1, :], MODB[:, 1, :], 1.0, gbc[:], ALU.add, ALU.mult),
                     reads=[modb_b, gbc_b], writes=[modb_b])

        def emit_norm(xap, x_b, G, SH, g_b, junk, junk_b, ss, ss_b, hn, hn_b, h16, h16_b):
            act.emit(lambda e: e.activation(junk[:], xap, AF.Square, accum_out=ss[:]), reads=[x_b], writes=[junk_b, ss_b])
            dve.emit(lambda e: e.tensor_scalar(ss[:], ss[:], 1.0 / D, EPS, ALU.mult, ALU.add), reads=[ss_b], writes=[ss_b])
            act.emit(lambda e: e.activation(ss[:], ss[:], AF.Sqrt), reads=[ss_b], writes=[ss_b])
            dve.emit(lambda e: e.reciprocal(ss[:], ss[:]), reads=[ss_b], writes=[ss_b])
            dve.emit(lambda e: e.scalar_tensor_tensor(hn[:], xap, ss[:], G, ALU.mult, ALU.mult), reads=[x_b, ss_b, g_b], writes=[hn_b])
            if SH is not None:
                pool.emit(lambda e: e.tensor_tensor(h16[:], hn[:], SH, ALU.add), reads=[hn_b, g_b], writes=[h16_b])

        def interleave(gens, weights):
            gens = list(gens)
            alive = [True] * len(gens)
            while any(alive):
                for gi, g in enumerate(gens):
                    if not alive[gi]:
                        continue
                    for _ in range(weights[gi]):
                        try:
                            next(g)
                        except StopIteration:
                            alive[gi] = False
                            break

        HT0 = nc.dram_tensor("ht_scratch", [NTILE, 128, 8, 128], BF16)
        ht_b = [Buf() for _ in range(NTILE)]
        d_ht = DSemPool(fw, "ht", 4)
        with ExitStack() as L0:
            G1V = sb(L0, "G1V", [128, D], F32); g1v_b = Buf()
            rmask = sb(L0, "rmask", [128, 4, 128], F32); rmask_b = Buf()
            VAL = sb(L0, "VAL", [128, NTILE], F32); val_b = Buf()
            fw.dma(sp, rmask[:], rmask_d.rearrange("p (h c) -> p h c", h=4), d_const, writes=[rmask_b])
            fw.dma(sp, VAL[:], valid_d[:, :], d_const, writes=[val_b])
            with ExitStack() as PP0:
                MODA = sb(PP0, "MODA", [128, 3, D], F32); moda_b = Buf()
                compute_mod(0, 0, MODA, moda_b)
                dve.emit(lambda e: e.tensor_copy(G1V[:], MODA[:, 2, :]), reads=[moda_b], writes=[g1v_b])
                NB = 3
                xt = [sb(PP0, f"pxt{i}", [128, D], F32) for i in range(NB)]; xt_b = [Buf() for _ in range(NB)]
                sq = [sb(PP0, f"psq{i}", [128, D], BF16) for i in range(2)]; sq_b = [Buf(), Buf()]
                ssv = [sb(PP0, f"pss{i}", [128, 1], F32) for i in range(NB)]; ss_b = [Buf() for _ in range(NB)]
                hn = [sb(PP0, f"phn{i}", [128, D], F32) for i in range(2)]; hn_b = [Buf(), Buf()]
                h16 = [sb(PP0, f"ph16{i}", [128, D], BF16) for i in range(2)]; h16_b = [Buf(), Buf()]
                hTo = [sb(PP0, f"phT{i}", [128, 8, 128], BF16) for i in range(2)]; hTo_b = [Buf(), Buf()]
                PTP = [ps(PP0, f"PTP{i}", [128, 8, 128], BF16) for i in range(2)]; PTP_b = [Buf(), Buf()]

                def pp_ld(j):
                    i = j % NB
                    fw.dma(sp, xt[i][:], xext[j * 128:(j + 1) * 128, :], d_xp[i], writes=[xt_b[i]])

                def pp_s0(j):
                    i = j % NB
                    if j + 1 < NTILE:
                        pp_ld(j + 1)
                    act.emit(lambda e: e.activation(sq[j % 2][:], xt[i][:], AF.Square, accum_out=ssv[i][:]), reads=[xt_b[i]], writes=[sq_b[j % 2], ss_b[i]])
                    yield

                def pp_s1(j):
                    i = j % NB
                    k = j % 2
                    dve.emit(lambda e: e.tensor_scalar(ssv[i][:], ssv[i][:], 1.0 / D, EPS, ALU.mult, ALU.add), reads=[ss_b[i]], writes=[ss_b[i]])
                    act.emit(lambda e: e.activation(ssv[i][:], ssv[i][:], AF.Sqrt), reads=[ss_b[i]], writes=[ss_b[i]])
                    yield
                    dve.emit(lambda e: e.reciprocal(ssv[i][:], ssv[i][:]), reads=[ss_b[i]], writes=[ss_b[i]])
                    dve.emit(lambda e: e.scalar_tensor_tensor(hn[k][:], xt[i][:], ssv[i][:], MODA[:, 1, :], ALU.mult, ALU.mult), reads=[xt_b[i], ss_b[i], moda_b], writes=[hn_b[k]])
                    (pool if j % 2 == 0 else dve).emit(lambda e: e.tensor_tensor(h16[k][:], hn[k][:], MODA[:, 0, :], ALU.add), reads=[hn_b[k], moda_b], writes=[h16_b[k]])
                    yield

                def pp_s2(j):
                    k = j % 2
                    for kc in range(8):
                        pe.emit(lambda e: e.transpose(PTP[k][:, kc, :], h16[k][:, kc * 128:(kc + 1) * 128], ident[:]),
                                reads=[h16_b[k], ident_b], writes=[PTP_b[k]], signal=(kc == 7))
                    act.emit(lambda e: e.copy(hTo[k][:], PTP[k][:]), reads=[PTP_b[k]], writes=[hTo_b[k]])
                    fw.dma(act, HT0[j], hTo[k][:], d_ht, reads=[hTo_b[k]], writes=[ht_b[j]])
                    yield

                d_xp = [fw.dsem(f"xp{i}") for i in range(NB)]
                pp_ld(0)
                bg = mod_rows_gen(PP0, [(0, 1), (1, 0), (1, 1)])
                bg_alive = True
                for it in range(NTILE + 2):
                    gens = []
                    if 0 <= it - 1 < NTILE:
                        gens.append(pp_s1(it - 1))
                    if it < NTILE:
                        gens.append(pp_s0(it))
                    if it - 2 >= 0:
                        gens.append(pp_s2(it - 2))
                    for g_ in gens:
                        for _ in g_:
                            pass
                    if bg_alive:
                        for _ in range(2):
                            try:
                                next(bg)
                            except StopIteration:
                                bg_alive = False
                                break
                while bg_alive:
                    try:
                        next(bg)
                    except StopIteration:
                        bg_alive = False
                fw.barrier()

            for grp in range(2):
                with ExitStack() as G:
                    WIN = sb(G, "WIN", [128, 8, 3072], BF16); win_seg_b = [Buf() for _ in range(4)]
                    WOUT = sb(G, "WOUT", [128, 8, D], BF16); wout_b = Buf()
                    QDB = sb(G, "QDB", [128, 512], F32); qdb_b = Buf()
                    KDB = sb(G, "KDB", [128, 512], F32); kdb_b = Buf()
                    S32 = sb(G, "S32", [128, 2, 2, 512], F32); s32_b = [[Buf(), Buf()], [Buf(), Buf()]]
                    S16 = sb(G, "S16", [128, 2, 2, 512], BF16); s16_b = [[Buf(), Buf()], [Buf(), Buf()]]
                    wv = ret_w_in[0].rearrange("(k p) n -> p k n", p=128)
                    segs = [(1024 + grp * 512, 512, 512), (2048 + grp * 1024, 1024, 1024), (grp * 512, 512, 0), (4096 + grp * 1024, 1024, 2048)]
                    d_wins = [d_win, d_pb[0], d_pb[1], d_pb[2]]
                    for si, (src, n, dst) in enumerate(segs):
                        fw.dma(pool, WIN[:, :, dst:dst + n], wv[:, :, src:src + n], d_wins[si], writes=[win_seg_b[si]])

                    def win_buf(col0):
                        if col0 < 512:
                            return win_seg_b[2]
                        if col0 < 1024:
                            return win_seg_b[0]
                        if col0 < 2048:
                            return win_seg_b[1]
                        return win_seg_b[3]
                    fw.dma(sp, QDB[:], qdb_d[:, grp * 512:(grp + 1) * 512], d_const, writes=[qdb_b])
                    fw.dma(sp, KDB[:], kdb_d[:, grp * 512:(grp + 1) * 512], d_const, writes=[kdb_b])
                    with ExitStack() as st:
                        WST = sb(st, "WST", [128, 4, D], F32); wst_b = Buf()
                        gn = sb(st, "gn", [128, 8], F32); gn_b = Buf()
                        fw.dma(sp, gn[:], ret_gn[0, grp * 1024:(grp + 1) * 1024].rearrange("(k p) -> p k", p=128), d_const,
                               writes=[gn_b], allow_slow_non_contiguous=True)
                        wo = ret_w_out[0, grp * 1024:(grp + 1) * 1024, :].rearrange("(k p) n -> p k n", p=128)
                        for hf in range(2):
                            fw.dma(sp, WST[:], wo[:, hf * 4:(hf + 1) * 4, :], d_w[1], writes=[wst_b])
                            for k4 in range(4):
                                kc = hf * 4 + k4
                                dve.emit(lambda e: e.scalar_tensor_tensor(WOUT[:, kc, :], WST[:, k4, :], gn[:, kc:kc + 1], G1V[:], ALU.mult, ALU.mult),
                                         reads=[wst_b, gn_b, g1v_b], writes=[wout_b])
                        fw.sync_on([(dve.sem, dve.count)])
                    dve.emit(lambda e: e.memset(S32[:], 0.0), writes=s32_b[0] + s32_b[1])
                    pool.emit(lambda e: e.memset(S16[:], 0.0), writes=s16_b[0] + s16_b[1])

                    with ExitStack() as T:
                        hTt = [sb(T, f"hTt{i}", [128, 8, 128], BF16) for i in range(2)]; hTt_b = [Buf(), Buf()]
                        xt = [sb(T, f"xt{i}", [128, D], F32) for i in range(2 if grp == 0 else 0)]; xt_b = [Buf(), Buf()]
                        cst = [sb(T, f"cst{i}", [128, 256], F32) for i in range(2)]; cst_b = [Buf(), Buf()]
                        tmp = [sb(T, f"rt{i}", [128, 2, 128], F32) for i in range(4)]; tmp_b = [Buf() for _ in range(4)]
                        rotq = sb(T, "rotq", [128, 2, 256], BF16); rotq_b = Buf()
                        rotk = [sb(T, f"rotk{i}", [128, 2, 256], BF16) for i in range(2)]; rotk_b = [Buf(), Buf()]
                        qd = sb(T, "qd", [128, 512], BF16); qd_b = Buf()
                        ktok = [sb(T, f"ktok{i}", [128, 512], BF16) for i in range(2)]; ktok_b = [Buf(), Buf()]
                        qkT = [sb(T, f"qkT{i}", [128, 8, 128], BF16) for i in range(2)]; qkT_b = [Buf(), Buf()]
                        v16 = [sb(T, f"v16{i}", [128, 1024], BF16) for i in range(2)]; v16_b = [[Buf(), Buf()], [Buf(), Buf()]]
                        sg = [sb(T, f"sg{i}", [128, 1024], BF16) for i in range(2)]; sg_b = [[Buf(), Buf()], [Buf(), Buf()]]
                        sT16 = sb(T, "sT16", [128, 2, 128], BF16); sT_b = [Buf(), Buf()]
                        on32_0 = sb(T, "on32", [128, 512], F32); on32_0b = Buf()
                        on32 = [on32_0, on32_0]; on_b = [on32_0b, on32_0b]
                        y16s = [sb(T, f"y16_{i}", [128, 1024], BF16) for i in range(2)]; y16s_b = [[Buf(), Buf()], [Buf(), Buf()]]
                        yT = sb(T, "yT", [128, 8, 128], BF16); yT_b = Buf()
                        bst_t = [sb(T, f"bst{i}", [128, 32], F32) for i in range(2)]; bst_b = [Buf(), Buf()]
                        mv_t = [sb(T, f"mv{i}", [128, 32], F32) for i in range(2)]; mv_b = [Buf(), Buf()]
                        bst = [t_[:, 0:6] for t_ in bst_t]
                        mv = [t_[:, 0:2] for t_ in mv_t]
                        PJ = [ps(T, f"PJ{i}", [128, 512], F32) for i in range(NPJ)]; PJ_b = [Buf() for _ in range(NPJ)]
                        PTA = ps(T, "PTA", [128, 8, 128], BF16); PTA_b = Buf()
                        PTY = ps(T, "PTY", [128, 8, 128], BF16); PTY_b = Buf()
                        PSC = ps(T, "PSC", [128, 2, 128], F32); PSC_b = [Buf(), Buf()]
                        if NPJ == 4:
                            PO0 = ps(T, "PO0", [128, 512], F32); PO0_b = Buf()
                            PO = [PO0, PO0]; PO_b = [PO0_b, PO0_b]
                        else:
                            PO = [ps(T, f"PO{i}", [128, 512], F32) for i in range(2)]; PO_b = [Buf(), Buf()]
                        pj_ctr = [0]
                        d_xr = [fw.dsem(f"xr{grp}{i}") for i in range(2)]

                        def next_pj():
                            i = pj_ctr[0] % NPJ
                            pj_ctr[0] += 1
                            return PJ[i], PJ_b[i]

                        def proj(hT_, hT_bb, col0, ncol):
                            P, P_b = next_pj()
                            for kc in range(8):
                                pe.emit(lambda e: e.matmul(P[:, 0:ncol], hT_[:, kc, :], WIN[:, kc, col0:col0 + ncol], start=(kc == 0), stop=(kc == 7)),
                                        reads=[hT_bb, win_buf(col0)], writes=[P_b], signal=(kc == 7))
                            return P, P_b

                        def rotary(P, P_b, cs_t, cs_b, rot, rot_b):
                            X = P[:, :].rearrange("p (h t i) -> p h t i", h=2, t=2)
                            X1 = X[:, :, 0, :]
                            X2 = X[:, :, 1, :]
                            cosb = cs_t[:, 0:128].unsqueeze(1).to_broadcast([128, 2, 128])
                            sinb = cs_t[:, 128:256].unsqueeze(1).to_broadcast([128, 2, 128])
                            dve.emit(lambda e: e.tensor_tensor(tmp[0][:], X1, cosb, ALU.mult), reads=[P_b, cs_b], writes=[tmp_b[0]])
                            dve.emit(lambda e: e.tensor_tensor(tmp[1][:], X2, sinb, ALU.mult), reads=[P_b, cs_b], writes=[tmp_b[1]])
                            pool.emit(lambda e: e.tensor_tensor(rot[:, :, 0:128], tmp[0][:], tmp[1][:], ALU.subtract), reads=[tmp_b[0], tmp_b[1]], writes=[rot_b])
                            yield
                            dve.emit(lambda e: e.tensor_tensor(tmp[2][:], X1, sinb, ALU.mult), reads=[P_b, cs_b], writes=[tmp_b[2]])
                            dve.emit(lambda e: e.tensor_tensor(tmp[3][:], X2, cosb, ALU.mult), reads=[P_b, cs_b], writes=[tmp_b[3]])
                            pool.emit(lambda e: e.tensor_tensor(rot[:, :, 128:256], tmp[2][:], tmp[3][:], ALU.add), reads=[tmp_b[2], tmp_b[3]], writes=[rot_b])
                            yield

                        first_deep = (NDEEP - 8) if grp == 0 else 0

                        def stage2(j):
                            full = j >= HALO
                            bi = j % 2
                            fw.dma(sp, hTt[bi][:], HT0[j], d_x[bi], reads=[ht_b[j]], writes=[hTt_b[bi]])
                            hTt_b[bi].lw = None
                            fw.dma(sp, cst[bi][:], cs_d[j * 128:(j + 1) * 128, :], d_x[bi], writes=[cst_b[bi]])
                            hTt_b[bi].lw = cst_b[bi].lw
                            yield
                            Pk, Pk_b = proj(hTt[bi], hTt_b[bi], 512, 512)
                            yield
                            yield from rotary(Pk, Pk_b, cst[bi], cst_b[bi], rotk[bi], rotk_b[bi])
                            dve.emit(lambda e: e.scalar_tensor_tensor(ktok[bi][:], rotk[bi][:].rearrange("p h d -> p (h d)"), VAL[:, j:j + 1], KDB[:], ALU.mult, ALU.mult),
                                     reads=[rotk_b[bi], val_b, kdb_b], writes=[ktok_b[bi]])
                            yield
                            for hh in range(2):
                                Pv, Pv_b = proj(hTt[bi], hTt_b[bi], 1024 + hh * 512, 512)
                                act.emit(lambda e: e.copy(v16[bi][:, hh * 512:(hh + 1) * 512], Pv[:]), reads=[Pv_b], writes=[v16_b[bi][hh]])
                                yield
                            if full:
                                Pq, Pq_b = proj(hTt[bi], hTt_b[bi], 0, 512)
                                yield
                                yield from rotary(Pq, Pq_b, cst[bi], cst_b[bi], rotq, rotq_b)
                                pool.emit(lambda e: e.tensor_tensor(qd[:], rotq[:].rearrange("p h d -> p (h d)"), QDB[:], ALU.mult),
                                          reads=[rotq_b, qdb_b], writes=[qd_b])
                                yield
                                for hh in range(2):
                                    Pg, Pg_b = proj(hTt[bi], hTt_b[bi], 2048 + hh * 512, 512)
                                    act.emit(lambda e: e.activation(sg[bi][:, hh * 512:(hh + 1) * 512], Pg[:], AF.Silu), reads=[Pg_b], writes=[sg_b[bi][hh]])
                                    yield
                                rk = rotk[bi][:].rearrange("p h d -> p (h d)")
                                for blk in range(4):
                                    pe.emit(lambda e: e.transpose(PTA[:, blk, :], qd[:, blk * 128:(blk + 1) * 128], ident[:]),
                                            reads=[qd_b, ident_b], writes=[PTA_b], signal=False)
                                for blk in range(4):
                                    pe.emit(lambda e: e.transpose(PTA[:, 4 + blk, :], rk[:, blk * 128:(blk + 1) * 128], ident[:]),
                                            reads=[rotk_b[bi], ident_b], writes=[PTA_b], signal=(blk == 3))
                                act.emit(lambda e: e.copy(qkT[bi][:], PTA[:]), reads=[PTA_b], writes=[qkT_b[bi]])
                                yield

                        def head_chain(j, hh):
                            full = j >= HALO
                            bi = j % 2
                            h = grp * 2 + hh
                            if full:
                                for half in range(2):
                                    pe.emit(lambda e: e.matmul(PSC[:, hh, :], qkT[bi][:, 4 + hh * 2 + half, :], qkT[bi][:, hh * 2 + half, :], start=(half == 0), stop=(half == 1)),
                                            reads=[qkT_b[bi]], writes=[PSC_b[0], PSC_b[1]], signal=(half == 1))
                                dve.emit(lambda e: e.tensor_tensor(sT16[:, hh, :], PSC[:, hh, :], rmask[:, h, :], ALU.mult),
                                         reads=[PSC_b[hh], rmask_b], writes=[sT_b[hh]])
                                yield
                                pe.emit(lambda e: e.matmul(PO[hh][:], sT16[:, hh, :], v16[bi][:, hh * 512:(hh + 1) * 512], start=True, stop=False),
                                        reads=[sT_b[hh], v16_b[bi][hh]], writes=[PO_b[hh]], signal=False)
                                for half in range(2):
                                    pe.emit(lambda e: e.matmul(PO[hh][:], qkT[bi][:, hh * 2 + half, :], S16[:, hh, half, :], start=False, stop=(half == 1)),
                                            reads=[qkT_b[bi], s16_b[hh][half]], writes=[PO_b[hh]], signal=(half == 1))
                                dve.emit(lambda e: e.bn_stats(bst[hh], PO[hh][:]), reads=[PO_b[hh]], writes=[bst_b[hh]])
                                dve.emit(lambda e: e.bn_aggr(mv[hh], bst[hh]), reads=[bst_b[hh]], writes=[mv_b[hh]])
                                yield
                                act.emit(lambda e: e.activation(mv[hh][:, 1:2], mv[hh][:, 1:2], AF.Sqrt, bias=eps_t[:]), reads=[mv_b[hh], eps_b], writes=[mv_b[hh]])
                                dve.emit(lambda e: e.reciprocal(mv[hh][:, 1:2], mv[hh][:, 1:2]), reads=[mv_b[hh]], writes=[mv_b[hh]])
                                dve.emit(lambda e: e.tensor_scalar(on32[hh][:], PO[hh][:], mv[hh][:, 0:1], mv[hh][:, 1:2], ALU.subtract, ALU.mult),
                                         reads=[PO_b[hh], mv_b[hh]], writes=[on_b[hh]])
                                pool.emit(lambda e: e.tensor_tensor(y16s[bi][:, hh * 512:(hh + 1) * 512], on32[hh][:], sg[bi][:, hh * 512:(hh + 1) * 512], ALU.mult),
                                          reads=[on_b[hh], sg_b[bi][hh]], writes=[y16s_b[bi][hh]])
                                yield
                            gC = float(GAMMA[h] ** 128)
                            for half in range(2):
                                Pkv, Pkv_b = next_pj()
                                pe.emit(lambda e: e.matmul(Pkv[:], ktok[bi][:, hh * 256 + half * 128: hh * 256 + (half + 1) * 128], v16[bi][:, hh * 512:(hh + 1) * 512], start=True, stop=True),
                                        reads=[ktok_b[bi], v16_b[bi][hh]], writes=[Pkv_b])
                                dve.emit(lambda e: e.scalar_tensor_tensor(S32[:, hh, half, :], S32[:, hh, half, :], gC, Pkv[:], ALU.mult, ALU.add),
                                         reads=[Pkv_b, s32_b[hh][half]], writes=[s32_b[hh][half]])
                                if j < NTILE - 1:
                                    act.emit(lambda e: e.copy(S16[:, hh, half, :], S32[:, hh, half, :]), reads=[s32_b[hh][half]], writes=[s16_b[hh][half]])
                                yield

                        def stage3(j):
                            full = j >= HALO
                            bi = j % 2
                            if full and grp == 0:
                                fw.dma(sp, xt[bi][:], xext[j * 128:(j + 1) * 128, :], d_xr[bi], writes=[xt_b[bi]])
                            g0 = head_chain(j, 0)
                            g1 = head_chain(j, 1)
                            if SEQ_HEADS:
                                for _ in g0:
                                    yield
                                for _ in g1:
                                    yield
                            else:
                                alive = [True, True]
                                if full:
                                    next(g0)
                                    yield
                                while any(alive):
                                    for gi, g in enumerate((g0, g1)):
                                        if alive[gi]:
                                            try:
                                                next(g)
                                            except StopIteration:
                                                alive[gi] = False
                                    yield
                        def stage4(j):
                            full = j >= HALO
                            bi = j % 2
                            if full:
                                ti = j - HALO
                                for kc in range(8):
                                    pe.emit(lambda e: e.transpose(PTY[:, kc, :], y16s[bi][:, kc * 128:(kc + 1) * 128], ident[:]),
                                            reads=[y16s_b[bi][kc // 4], ident_b], writes=[PTY_b], signal=(kc == 7))
                                act.emit(lambda e: e.copy(yT[:], PTY[:]), reads=[PTY_b], writes=[yT_b])
                                yield
                                for ch in range(2):
                                    Pm, Pm_b = next_pj()
                                    for kc in range(8):
                                        pe.emit(lambda e: e.matmul(Pm[:], yT[:, kc, :], WOUT[:, kc, ch * 512:(ch + 1) * 512], start=(kc == 0), stop=(kc == 7)),
                                                reads=[yT_b, wout_b], writes=[Pm_b], signal=(kc == 7))
                                    xr = XRES[:, ti, ch * 512:(ch + 1) * 512]
                                    if grp == 0:
                                        dve.emit(lambda e: e.tensor_tensor(xr, Pm[:], xt[bi][:, ch * 512:(ch + 1) * 512], ALU.add),
                                                 reads=[Pm_b, xt_b[bi]], writes=[xres_b[ti]])
                                    else:
                                        dve.emit(lambda e: e.tensor_tensor(xr, Pm[:], xr, ALU.add), reads=[Pm_b, xres_b[ti]], writes=[xres_b[ti]])
                                    yield

                            return
                            yield

                        steps = list(range(first_deep, NTILE))
                        for it in range(len(steps) + 2):
                            gens = []
                            wts = []
                            if it - 2 >= 0:
                                gens.append(stage4(steps[it - 2])); wts.append(1)
                            if RET_ORDER == 0:
                                if 0 <= it - 1 < len(steps):
                                    gens.append(stage3(steps[it - 1])); wts.append(RET_W3)
                                if it < len(steps):
                                    gens.append(stage2(steps[it])); wts.append(RET_W2)
                            else:
                                if it < len(steps):
                                    gens.append(stage2(steps[it])); wts.append(RET_W2)
                                if 0 <= it - 1 < len(steps):
                                    gens.append(stage3(steps[it - 1])); wts.append(RET_W3)
                            interleave(gens, wts)
                        fw.barrier()
            fw.barrier()

        def ffn_block(stack, hT, hT_b, ntok, wg_ap, wu_ap, wd_ap, nf, groups, evac, tag, state=None):
            gmax = max(j1 - j0 for j0, j1 in groups)
            pieces = []
            t0 = 0
            while t0 < ntok:
                n = min(512, ntok - t0)
                pieces.append((t0, n))
                t0 += n
            ntl = ntok // 128
            if state is None:
                state = {}
            if "aT" not in state:
                state["aT"] = sb(stack, "aT" + tag, [128, gmax, ntok], BF16); state["aT_b"] = [Buf() for _ in range(gmax)]
                state["WDq"] = [sb(stack, f"WDq{i}" + tag, [128, gmax, D], BF16) for i in range(2)]; state["WDq_b"] = [Buf(), Buf()]
                state["wgu"] = [sb(stack, f"wgu{i}" + tag, [128, 8, 2, 256], BF16) for i in range(2)]; state["wgu_b"] = [Buf(), Buf()]
                state["sgt"] = [sb(stack, f"sgt{i}" + tag, [128, 512], F32) for i in range(2)]; state["sgt_b"] = [Buf(), Buf()]
                state["PG"] = [ps(stack, f"PG{i}" + tag, [128, 512], F32) for i in range(2)]; state["PG_b"] = [Buf(), Buf()]
                state["PU"] = [ps(stack, f"PU{i}" + tag, [128, 512], F32) for i in range(2)]; state["PU_b"] = [Buf(), Buf()]
                state["PD"] = [ps(stack, f"PD{i}" + tag, [128, 512], F32) for i in range(3)]; state["PD_b"] = [Buf() for _ in range(3)]
                state["d_wg"] = [fw.dsem(f"wgu{i}" + tag) for i in range(2)]
                state["d_wd"] = [fw.dsem(f"wd{i}" + tag) for i in range(2)]
                state["ctr"] = [0, 0, 0, 0]
            aT, aT_b, WDq, WDq_b, wgu, wgu_b = state["aT"], state["aT_b"], state["WDq"], state["WDq_b"], state["wgu"], state["wgu_b"]
            sgt, sgt_b, PG, PG_b, PU, PU_b, PD, PD_b = state["sgt"], state["sgt_b"], state["PG"], state["PG_b"], state["PU"], state["PU_b"], state["PD"], state["PD_b"]
            d_wg, d_wd = state["d_wg"], state["d_wd"]
            wgv, wuv, wdv = wg_ap, wu_ap, wd_ap
            ctr = state["ctr"]
            for gi, (j0, j1) in enumerate(groups):
                ng = j1 - j0
                wb = ctr[3] % 2
                ctr[3] += 1
                fw.dma(pool, WDq[wb][:, 0:ng, :], wdv[:, j0:j1, :], d_wd[wb], writes=[WDq_b[wb]])
                j = j0
                while j < j1:
                    nb = min(2, j1 - j)
                    bi = ctr[0] % 2
                    ctr[0] += 1
                    fw.dma(pool, wgu[bi][:, :, 0, 0:nb * 128], wgv[:, :, j * 128:(j + nb) * 128], d_wg[bi], writes=[wgu_b[bi]])
                    wgu_b[bi].lw = None
                    fw.dma(pool, wgu[bi][:, :, 1, 0:nb * 128], wuv[:, :, j * 128:(j + nb) * 128], d_wg[bi], writes=[wgu_b[bi]])
                    for ch in range(nb):
                        jj = j + ch - j0
                        for (p0, pn) in pieces:
                            pi = ctr[1] % 2
                            ctr[1] += 1
                            for which, (P, P_b) in enumerate(((PG[pi], PG_b[pi]), (PU[pi], PU_b[pi]))):
                                for kc in range(8):
                                    pe.emit(lambda e: e.matmul(P[:, 0:pn], wgu[bi][:, kc, which, ch * 128:(ch + 1) * 128], hT[:, kc, p0:p0 + pn], start=(kc == 0), stop=(kc == 7)),
                                            reads=[wgu_b[bi], hT_b], writes=[P_b], signal=(kc == 7))
                            act.emit(lambda e: e.activation(sgt[pi][:, 0:pn], PG[pi][:, 0:pn], AF.Silu), reads=[PG_b[pi]], writes=[sgt_b[pi]])
                            dve.emit(lambda e: e.tensor_tensor(aT[:, jj, p0:p0 + pn], PU[pi][:, 0:pn], sgt[pi][:, 0:pn], ALU.mult),
                                     reads=[PU_b[pi], sgt_b[pi]], writes=[aT_b[jj]])
                    j += nb
                for tl in range(ntl):
                    for half in range(2):
                        di = ctr[2] % 3
                        ctr[2] += 1
                        for jj in range(ng):
                            pe.emit(lambda e: e.matmul(PD[di][:], aT[:, jj, tl * 128:(tl + 1) * 128], WDq[wb][:, jj, half * 512:(half + 1) * 512], start=(jj == 0), stop=(jj == ng - 1)),
                                    reads=[aT_b[jj], WDq_b[wb]], writes=[PD_b[di]], signal=(jj == ng - 1))
                        evac(tl, half, PD[di], PD_b[di], gi == 0)

        if upto != "ret":
            with ExitStack() as F0:
                MODB = sb(F0, "MODB", [128, 3, D], F32); modb_b = Buf()
                load_mod(0, 1, MODB, modb_b, F0)
                NT0 = 17
                hTa = sb(F0, "hTa", [128, 8, NT0 * 128], BF16); hTa_b = Buf()
                with ExitStack() as T:
                    ssv = [sb(T, f"fss{i}", [128, 16], F32) for i in range(3)]; ss_b = [Buf() for _ in range(3)]
                    hn = [sb(T, f"fhn{i}", [128, D], F32) for i in range(3)]; hn_b = [Buf() for _ in range(3)]
                    h16 = [sb(T, f"fh16{i}", [128, D], BF16) for i in range(2)]; h16_b = [Buf(), Buf()]
                    PTA = [ps(T, f"PTA{i}", [128, 8, 128], BF16) for i in range(2)]; PTA_b = [Buf(), Buf()]

                    def fa(t):
                        i = t % 3
                        act.emit(lambda e: e.activation(hn[i][:], XRES[:, t, :], AF.Square, accum_out=ssv[i][:, 0:1]), reads=[xres_b[t]], writes=[hn_b[i], ss_b[i]])
                        dve.emit(lambda e: e.tensor_scalar(ssv[i][:, 0:1], ssv[i][:, 0:1], 1.0 / D, EPS, ALU.mult, ALU.add), reads=[ss_b[i]], writes=[ss_b[i]])
                        yield

                    def fb(t):
                        i = t % 3
                        k = t % 2
                        act.emit(lambda e: e.activation(ssv[i][:, 0:1], ssv[i][:, 0:1], AF.Sqrt), reads=[ss_b[i]], writes=[ss_b[i]])
                        dve.emit(lambda e: e.reciprocal(ssv[i][:, 0:1], ssv[i][:, 0:1]), reads=[ss_b[i]], writes=[ss_b[i]])
                        dve.emit(lambda e: e.scalar_tensor_tensor(hn[i][:], XRES[:, t, :], ssv[i][:, 0:1], MODB[:, 1, :], ALU.mult, ALU.mult),
                                 reads=[xres_b[t], ss_b[i], modb_b], writes=[hn_b[i]])
                        (pool if t % 2 == 0 else dve).emit(lambda e: e.tensor_tensor(h16[k][:], hn[i][:], MODB[:, 0, :], ALU.add), reads=[hn_b[i], modb_b], writes=[h16_b[k]])
                        yield

                    def fc(t):
                        k = t % 2
                        for kc in range(8):
                            pe.emit(lambda e: e.transpose(PTA[k][:, kc, :], h16[k][:, kc * 128:(kc + 1) * 128], ident[:]),
                                    reads=[h16_b[k], ident_b], writes=[PTA_b[k]], signal=(kc == 7))
                        act.emit(lambda e: e.copy(hTa[:, :, t * 128:(t + 1) * 128], PTA[k][:]), reads=[PTA_b[k]], writes=[hTa_b])
                        yield

                    for it in range(NT0 + 2):
                        gens = []
                        if 0 <= it - 1 < NT0:
                            gens.append(fb(it - 1))
                        if it < NT0:
                            gens.append(fa(it))
                        if 0 <= it - 2 < NT0:
                            gens.append(fc(it - 2))
                        for g_ in gens:
                            for _ in g_:
                                pass
                    fw.barrier()
                with ExitStack() as T:
                    etmp = [sb(T, f"etmp{i}", [128, 512], F32) for i in range(2)]; etmp_b = [Buf(), Buf()]
                    ectr = [0]

                    def evac0(tl, half, bank, bank_b, first):
                        i = ectr[0] % 2
                        ectr[0] += 1
                        xr = XRES[:, tl, half * 512:(half + 1) * 512]
                        dve.emit(lambda e: e.tensor_tensor(etmp[i][:], bank[:], MODB[:, 2, half * 512:(half + 1) * 512], ALU.mult),
                                 reads=[bank_b, modb_b], writes=[etmp_b[i]])
                        (pool if ectr[0] % 3 != 0 else dve).emit(lambda e: e.tensor_tensor(xr, xr, etmp[i][:], ALU.add), reads=[etmp_b[i], xres_b[tl]], writes=[xres_b[tl]])

                    ffn_block(T, hTa, hTa_b, NT0 * 128, ffn_wg[0].rearrange("(k p) n -> p k n", p=128), ffn_wu[0].rearrange("(k p) n -> p k n", p=128),
                              ffn_wd[0].rearrange("(j p) n -> p j n", p=128), NF, [(0, 6), (6, 12), (12, 17), (17, 22)], evac0, "f0")
                    fw.barrier()
                fw.barrier()

        if upto not in ("ret", "ffn"):
            with ExitStack() as P1:
                MODA1 = sb(P1, "MODA1", [128, 3, D], F32); moda1_b = Buf()
                load_mod(1, 0, MODA1, moda1_b, P1)
                WP = sb(P1, "WP", [128, 8, 256], BF16); wp_b = Buf()
                BPB = sb(P1, "BPB", [128, D], F32); bpb_b = Buf()
                SCG = sb(P1, "SCG", [128, D], F32); scg_b = Buf()
                BC = sb(P1, "BC", [128, 4, 128], BF16); bc_b = Buf()
                BP = sb(P1, "BP", [128, 4, 128], BF16); bp_b = Buf()
                BC0 = sb(P1, "BC0", [128, 4, 128], BF16); bc0_b = Buf()
                BP0 = sb(P1, "BP0", [128, 4, 128], BF16); bp0_b = Buf()
                fw.dma(pool, BC[:], pbc_d.rearrange("p (g t) -> p g t", g=4), d_pb[0], writes=[bc_b])
                fw.dma(pool, BP[:], pbp_d.rearrange("p (g t) -> p g t", g=4), d_pb[1], writes=[bp_b])
                fw.dma(pool, BC0[:], pbc0_d.rearrange("p (g t) -> p g t", g=4), d_pb[2], writes=[bc0_b])
                fw.dma(pool, BP0[:], pbp0_d.rearrange("p (g t) -> p g t", g=4), d_pb[3], writes=[bp0_b])
                fw.dma(sp, SCG[:], pool_scale[0, :].partition_broadcast(128), d_const, writes=[scg_b])
                fw.dma(sp, BPB[:], pool_b[0].rearrange("g c -> (g c)").partition_broadcast(128), d_const, writes=[bpb_b])
                dve.emit(lambda e: e.tensor_tensor(SCG[:], SCG[:], MODA1[:, 2, :], ALU.mult), reads=[scg_b, moda1_b], writes=[scg_b])
                dve.emit(lambda e: e.tensor_tensor(BPB[:], BPB[:], SCG[:], ALU.mult), reads=[scg_b, bpb_b], writes=[bpb_b])
                with ExitStack() as st:
                    WPS = sb(st, "WPS", [128, 8, 256], F32); wps_b = Buf()
                    fw.dma(sp, WPS[:], pool_w[0].rearrange("g (k p) d -> p (g k) d", p=128), d_const, writes=[wps_b])
                    for g in range(4):
                        for kc in range(2):
                            dve.emit(lambda e: e.tensor_tensor(WP[:, g * 2 + kc, :], WPS[:, g * 2 + kc, :], SCG[:, g * 256:(g + 1) * 256], ALU.mult),
                                     reads=[wps_b, scg_b], writes=[wp_b])
                    fw.barrier()
                with ExitStack() as T:
                    NH = 3
                    ssv = [sb(T, f"ss{i}", [128, 1], F32) for i in range(2)]; ss_b = [Buf(), Buf()]
                    hn = [sb(T, f"hn{i}", [128, D], F32) for i in range(2)]; hn_b = [Buf(), Buf()]
                    h16 = [sb(T, f"h16{i}", [128, D], BF16) for i in range(NH)]; h16_b = [Buf() for _ in range(NH)]
                    pT16 = [sb(T, f"pT16{i}", [128, 8, 128], BF16) for i in range(2)]; pT16_b = [Buf(), Buf()]
                    PP = [ps(T, f"PP{i}", [128, 8, 128], F32) for i in range(2)]; PP_b = [Buf(), Buf()]
                    PY = [ps(T, f"PY{i}", [128, D], F32) for i in range(2)]; PY_b = [Buf(), Buf()]

                    def pl_a(t):
                        k = t % 2
                        c = t % NH
                        act.emit(lambda e: e.activation(hn[k][:], XRES[:, t, :], AF.Square, accum_out=ssv[k][:]), reads=[xres_b[t]], writes=[hn_b[k], ss_b[k]])
                        dve.emit(lambda e: e.tensor_scalar(ssv[k][:], ssv[k][:], 1.0 / D, EPS, ALU.mult, ALU.add), reads=[ss_b[k]], writes=[ss_b[k]])
                        yield
                        act.emit(lambda e: e.activation(ssv[k][:], ssv[k][:], AF.Sqrt), reads=[ss_b[k]], writes=[ss_b[k]])
                        dve.emit(lambda e: e.reciprocal(ssv[k][:], ssv[k][:]), reads=[ss_b[k]], writes=[ss_b[k]])
                        yield
                        dve.emit(lambda e: e.scalar_tensor_tensor(hn[k][:], XRES[:, t, :], ssv[k][:], MODA1[:, 1, :], ALU.mult, ALU.mult),
                                 reads=[xres_b[t], ss_b[k], moda1_b], writes=[hn_b[k]])
                        (pool if t % 2 == 0 else dve).emit(lambda e: e.tensor_tensor(h16[c][:], hn[k][:], MODA1[:, 0, :], ALU.add), reads=[hn_b[k], moda1_b], writes=[h16_b[c]])
                        yield

                    def pl_b(t):
                        i = t % 2
                        c = t % NH
                        cp = (t - 1) % NH
                        Bcur, Bcur_b = (BC0, bc0_b) if t == 1 else (BC, bc_b)
                        Bprv, Bprv_b = (BP0, bp0_b) if t == 1 else (BP, bp_b)
                        for g in range(4):
                            for kc in range(2):
                                cs_ = slice(g * 256 + kc * 128, g * 256 + (kc + 1) * 128)
                                pe.emit(lambda e: e.matmul(PP[i][:, g * 2 + kc, :], h16[c][:, cs_], Bcur[:, g, :], start=True, stop=False),
                                        reads=[h16_b[c], Bcur_b], writes=[PP_b[i]], signal=False)
                                pe.emit(lambda e: e.matmul(PP[i][:, g * 2 + kc, :], h16[cp][:, cs_], Bprv[:, g, :], start=False, stop=True),
                                        reads=[h16_b[cp], Bprv_b], writes=[PP_b[i]], signal=(g == 3 and kc == 1))
                            if g == 1:
                                yield
                        act.emit(lambda e: e.copy(pT16[i][:], PP[i][:]), reads=[PP_b[i]], writes=[pT16_b[i]])
                        yield
                        for g in range(4):
                            pe.emit(lambda e: e.matmul(PY[i][:, g * 256:(g + 1) * 256], ones16[:], BPR16[:, g * 256:(g + 1) * 256], start=True, stop=False),
                                    reads=[ones16_b, bpr_b], writes=[PY_b[i]], signal=False)
                            for kc in range(2):
                                pe.emit(lambda e: e.matmul(PY[i][:, g * 256:(g + 1) * 256], pT16[i][:, g * 2 + kc, :], WP[:, g * 2 + kc, :], start=False, stop=(kc == 1)),
                                        reads=[pT16_b[i], wp_b], writes=[PY_b[i]], signal=(g == 3 and kc == 1))
                        dve.emit(lambda e: e.tensor_tensor(XRES[:, t, :], PY[i][:], XRES[:, t, :], ALU.add), reads=[PY_b[i], xres_b[t]], writes=[xres_b[t]])
                        yield

                    ones16 = sb(T, "ones16", [1, 128], BF16); ones16_b = Buf()
                    BPR16 = sb(T, "BPR16", [1, D], BF16); bpr_b = Buf()
                    pool.emit(lambda e: e.memset(ones16[:], 1.0), writes=[ones16_b])
                    act.emit(lambda e: e.copy(BPR16[:], BPB[0:1, :]), reads=[bpb_b], writes=[bpr_b])
                    for it in range(17 + 1):
                        gens = []
                        if it < 17:
                            gens.append(pl_a(it))
                        if 1 <= it - 1 <= 16:
                            gens.append(pl_b(it - 1))
                        for g_ in gens:
                            for _ in g_:
                                pass
                    fw.barrier()
                fw.barrier()

        if upto == "all":
            ys_b = [Buf() for _ in range(NSLMAX)]
            d_ys = [fw.dsem(f"ys{i}") for i in range(NSLMAX)]; d_g = [[fw.dsem("g00"), fw.dsem("g01")], [fw.dsem("g10"), fw.dsem("g11")]]
            bc_reg = nc.gpsimd.alloc_register("bcreg")
            nc.gpsimd.reg_mov(bc_reg, TOTS - 1)
            with ExitStack() as M:
                G2V = sb(M, "G2V", [128, D], F32); g2v_b = Buf()
                GATE = sb(M, "GATE", [128, 16, 2], F32); gate_b = Buf()
                POS = sb(M, "POS", [128, 16, 2], mybir.dt.int32); pos_b = [Buf() for _ in range(16)]
                with ExitStack() as R:
                    MODB1 = sb(R, "MODB1", [128, 3, D], F32); modb1_b = Buf()
                    load_mod(1, 1, MODB1, modb1_b, R)
                    dve.emit(lambda e: e.tensor_copy(G2V[:], MODB1[:, 2, :]), reads=[modb1_b], writes=[g2v_b])
                    ident32 = sb(R, "ident32", [128, 128], F32); id32_b = Buf()
                    TRI = sb(R, "TRI", [128, 128], BF16); tri_b = Buf()
                    ONESM = sb(R, "ONESM", [128, 128], BF16); onesm_b = Buf()
                    MT = sb(R, "MT", [128, 96], F32); mt_b = Buf()
                    WR = sb(R, "WR", [128, 8, 8], F32); wr_b = Buf()
                    BASE = sb(R, "BASE", [128, 8], F32); base_b = Buf()
                    pool.emit(lambda e: e.memset(ident32[:], 1.0), writes=[id32_b])
                    pool.emit(lambda e: e.affine_select(ident32[:], ident32[:], [[-1, 128]], ALU.is_equal, 0.0, base=0, channel_multiplier=1), reads=[id32_b], writes=[id32_b])
                    pool.emit(lambda e: e.memset(TRI[:], 1.0), writes=[tri_b])
                    pool.emit(lambda e: e.affine_select(TRI[:], TRI[:], [[1, 128]], ALU.is_gt, 0.0, base=0, channel_multiplier=-1), reads=[tri_b], writes=[tri_b])
                    pool.emit(lambda e: e.memset(ONESM[:], 1.0), writes=[onesm_b])
                    pool.emit(lambda e: e.memset(BASE[:], 0.0), writes=[base_b])
                    fw.dma(sp, MT[:], moetab_d[:, :], d_const, writes=[mt_b])
                    fw.dma(sp, WR[:], moe_router[0].rearrange("(k p) e -> p k e", p=128), d_const, writes=[wr_b])
                    ssv = [sb(R, f"ssr{i}", [128, 1], F32) for i in range(2)]; ss_b = [Buf(), Buf()]
                    hn = [sb(R, f"hnr{i}", [128, D], F32) for i in range(2)]; hn_b = [Buf(), Buf()]
                    h32 = [sb(R, f"h32r{i}", [128, D], F32) for i in range(2)]; h32_b = [Buf(), Buf()]
                    H16 = sb(R, "H16all", [128, 16, D], BF16); h16_b = [Buf() for _ in range(16)]
                    hT32 = [sb(R, f"hT32{i}", [128, 8, 128], F32) for i in range(2)]; hT32_b = [Buf(), Buf()]
                    lg = [sb(R, f"lg{i}", [128, 8], F32) for i in range(2)]; lg_b = [Buf(), Buf()]
                    mx8 = sb(R, "mx8", [128, 8], F32); mx8_b = Buf()
                    A1 = sb(R, "A1all", [128, 16, 8], F32); a1_b = Buf()
                    A2 = sb(R, "A2all", [128, 16, 8], F32); a2_b = Buf()
                    RK = sb(R, "RKall", [128, 16, 8], F32); rk_b = Buf()
                    A16 = sb(R, "A16", [128, 8], BF16); a16_b = Buf()
                    PT32 = [ps(R, f"PT32{i}", [128, 8, 128], F32) for i in range(2)]; pt32_b = [Buf(), Buf()]
                    PLG = [ps(R, f"PLG{i}", [128, 8], F32) for i in range(2)]; plg_b = [Buf(), Buf()]
                    PPR = ps(R, "PPR", [128, 16], F32); ppr_b = Buf()

                    def r_a(ti):
                        t = ti + 1
                        k = ti % 2
                        act.emit(lambda e: e.activation(hn[k][:], XRES[:, t, :], AF.Square, accum_out=ssv[k][:]), reads=[xres_b[t]], writes=[hn_b[k], ss_b[k]])
                        dve.emit(lambda e: e.tensor_scalar(ssv[k][:], ssv[k][:], 1.0 / D, EPS, ALU.mult, ALU.add), reads=[ss_b[k]], writes=[ss_b[k]])
                        yield
                        act.emit(lambda e: e.activation(ssv[k][:], ssv[k][:], AF.Sqrt), reads=[ss_b[k]], writes=[ss_b[k]])
                        dve.emit(lambda e: e.reciprocal(ssv[k][:], ssv[k][:]), reads=[ss_b[k]], writes=[ss_b[k]])
                        yield
                        dve.emit(lambda e: e.scalar_tensor_tensor(hn[k][:], XRES[:, t, :], ssv[k][:], MODB1[:, 1, :], ALU.mult, ALU.mult),
                                 reads=[xres_b[t], ss_b[k], modb1_b], writes=[hn_b[k]])
                        (pool if ti % 2 == 0 else dve).emit(lambda e: e.tensor_tensor(h32[k][:], hn[k][:], MODB1[:, 0, :], ALU.add), reads=[hn_b[k], modb1_b], writes=[h32_b[k]])
                        act.emit(lambda e: e.copy(H16[:, ti, :], h32[k][:]), reads=[h32_b[k]], writes=[h16_b[ti]])
                        yield

                    def r_b(ti):
                        k = ti % 2
                        for kc in range(8):
                            pe.emit(lambda e: e.transpose(PT32[k][:, kc, :], h32[k][:, kc * 128:(kc + 1) * 128], ident32[:]),
                                    reads=[h32_b[k], id32_b], writes=[pt32_b[k]], signal=(kc == 7))
                        dve.emit(lambda e: e.tensor_copy(hT32[k][:], PT32[k][:]), reads=[pt32_b[k]], writes=[hT32_b[k]])
                        yield
                        for kc in range(8):
                            pe.emit(lambda e: e.matmul(PLG[k][:], hT32[k][:, kc, :], WR[:, kc, :], start=(kc == 0), stop=(kc == 7)),
                                    reads=[hT32_b[k], wr_b], writes=[plg_b[k]], signal=(kc == 7))
                        dve.emit(lambda e: e.tensor_copy(lg[k][:], PLG[k][:]), reads=[plg_b[k]], writes=[lg_b[k]])
                        yield

                    M12 = sb(R, "M12", [128, 16, 2], F32); m12_b = Buf()
                    pos8 = [sb(R, f"pos8{i}", [128, 8], F32) for i in range(2)]; pos8_b = [Buf(), Buf()]
                    ovf = [sb(R, f"ovf{i}", [128, 8], F32) for i in range(2)]; ovf_b = [Buf(), Buf()]
                    junk8 = sb(R, "junk8", [128, 8], F32); junk8_b = Buf()
                    posf = [sb(R, f"posf{i}", [128, 16], F32) for i in range(2)]; posf_b = [Buf(), Buf()]

                    def r_c(ti):
                        k = ti % 2
                        i = ti % 2
                        dve.emit(lambda e: e.max(mx8[:], lg[k][:]), reads=[lg_b[k]], writes=[mx8_b])
                        dve.emit(lambda e: e.tensor_scalar(A1[:, ti, :], lg[k][:], mx8[:, 0:1], None, ALU.is_equal), reads=[lg_b[k], mx8_b], writes=[a1_b])
                        dve.emit(lambda e: e.tensor_scalar(A2[:, ti, :], lg[k][:], mx8[:, 1:2], None, ALU.is_equal), reads=[lg_b[k], mx8_b], writes=[a2_b])
                        dve.emit(lambda e: e.tensor_tensor(A16[:], A1[:, ti, :], A2[:, ti, :], ALU.add), reads=[a1_b, a2_b], writes=[a16_b])
                        dve.emit(lambda e: e.tensor_copy(M12[:, ti, :], mx8[:, 0:2]), reads=[mx8_b], writes=[m12_b])
                        yield
                        pe.emit(lambda e: e.matmul(PPR[:, 0:8], TRI[:], A16[:], start=True, stop=True), reads=[tri_b, a16_b], writes=[ppr_b], signal=False)
                        pe.emit(lambda e: e.matmul(PPR[:, 8:16], ONESM[:], A16[:], start=True, stop=True), reads=[onesm_b, a16_b], writes=[ppr_b])
                        yield
                        dve.emit(lambda e: e.tensor_tensor(RK[:, ti, :], PPR[:, 0:8], BASE[:], ALU.add), reads=[ppr_b, base_b], writes=[rk_b])
                        dve.emit(lambda e: e.tensor_tensor(BASE[:], PPR[:, 8:16], BASE[:], ALU.add), reads=[ppr_b, base_b, rk_b], writes=[base_b])
                        dve.emit(lambda e: e.tensor_tensor(ovf[i][:], RK[:, ti, :], MT[:, 0:8], ALU.is_ge), reads=[rk_b, mt_b], writes=[ovf_b[i]])
                        dve.emit(lambda e: e.scalar_tensor_tensor(pos8[i][:], ovf[i][:], 1.0e6, RK[:, ti, :], ALU.mult, ALU.add), reads=[ovf_b[i], rk_b], writes=[pos8_b[i]])
                        dve.emit(lambda e: e.tensor_tensor(pos8[i][:], pos8[i][:], MT[:, 8:16], ALU.add), reads=[pos8_b[i], mt_b], writes=[pos8_b[i]])
                        for kk, (Ak, ak_b) in enumerate(((A1, a1_b), (A2, a2_b))):
                            dve.emit(lambda e: e.scalar_tensor_tensor(junk8[:], Ak[:, ti, :], 1.0, pos8[i][:], ALU.mult, ALU.mult, accum_out=posf[i][:, kk:kk + 1]),
                                     reads=[ak_b, pos8_b[i]], writes=[junk8_b, posf_b[i]])
                        dve.emit(lambda e: e.tensor_copy(POS[:, ti, :], posf[i][:, 0:2]), reads=[posf_b[i]], writes=[pos_b[ti]])
                        yield
                        for kk in range(2):
                            pool.deps([h16_b[ti], pos_b[ti]], [hs_b] if kk == 0 else [])
                            pool.obj.indirect_dma_start(out=HS[:, :], out_offset=bass.IndirectOffsetOnAxis(ap=POS[:, ti, kk:kk + 1], axis=0),
                                                        in_=H16[:, ti, :], in_offset=None, bounds_check=bc_reg, oob_is_err=False).then_inc(d_hs.sem, 16)
                            d_hs.count += 16
                            tok = (d_hs.sem, d_hs.count)
                            fw.note(tok, [h16_b[ti], pos_b[ti]], [])
                        yield

                    for it in range(16 + 2):
                        gens = []
                        if it < 16:
                            gens.append(r_a(it))
                        if 0 <= it - 1 < 16:
                            gens.append(r_b(it - 1))
                        if 0 <= it - 2 < 16:
                            gens.append(r_c(it - 2))
                        for g_ in gens:
                            for _ in g_:
                                pass
                    gd = sb(R, "gd", [128, 16], F32); gd_b = Buf()
                    ge = sb(R, "ge", [128, 16], F32); ge_b = Buf()
                    dve.emit(lambda e: e.tensor_tensor(gd[:], M12[:, :, 1], M12[:, :, 0], ALU.subtract), reads=[m12_b], writes=[gd_b])
                    act.emit(lambda e: e.activation(ge[:], gd[:], AF.Exp), reads=[gd_b], writes=[ge_b])
                    dve.emit(lambda e: e.tensor_scalar(gd[:], ge[:], 1.0, None, ALU.add), reads=[ge_b], writes=[gd_b])
                    dve.emit(lambda e: e.reciprocal(GATE[:, :, 0], gd[:]), reads=[gd_b], writes=[gate_b])
                    dve.emit(lambda e: e.tensor_tensor(GATE[:, :, 1], ge[:], GATE[:, :, 0], ALU.mult), reads=[ge_b, gate_b], writes=[gate_b])
                    hs_b.lw = (d_hs.sem, d_hs.count)
                    hs_b.rd = {}
                    fw.barrier()


                with ExitStack() as E:
                    hs = sb(E, "hs", [128, NSLMAX, D], BF16); hss_b = Buf()
                    hTe = sb(E, "hTe", [128, 8, CAPMAX], BF16); hTe_b = Buf()
                    YACC = sb(E, "YACC", [128, NSLMAX, D], F32); yacc_b = [Buf() for _ in range(NSLMAX)]
                    PTE = ps(E, "PTE", [128, 8, 128], BF16); pte_b = Buf()
                    groups = [(0, 7), (7, 14), (14, 21), (21, 28)]

                    def evacE(tl, half, bank, bank_b, first):
                        ya = YACC[:, tl, half * 512:(half + 1) * 512]
                        if first:
                            act.emit(lambda e: e.copy(ya, bank[:]), reads=[bank_b], writes=[yacc_b[tl]])
                        else:
                            dve.emit(lambda e: e.tensor_tensor(ya, bank[:], ya, ALU.add), reads=[bank_b, yacc_b[tl]], writes=[yacc_b[tl]])

                    with ExitStack() as FB:
                        fb_state = {}
                        for r in range(8):
                            capr = CAPS[r]; nslr = capr // 128; o0 = OFFS[r]
                            fw.dma(sp, hs[:, 0:nslr, :], HS[o0:o0 + capr, :].rearrange("(n p) d -> p n d", p=128), d_x[0], reads=[hs_b], writes=[hss_b])
                            for n in range(nslr):
                                for kc in range(8):
                                    pe.emit(lambda e: e.transpose(PTE[:, kc, :], hs[:, n, kc * 128:(kc + 1) * 128], ident[:]),
                                            reads=[hss_b, ident_b], writes=[pte_b], signal=(kc == 7))
                                act.emit(lambda e: e.copy(hTe[:, :, n * 128:(n + 1) * 128], PTE[:]), reads=[pte_b], writes=[hTe_b])
                            wg_v = moe_wg[0, r].rearrange("(k p) n -> p k n", p=128)
                            wu_v = moe_wu[0, r].rearrange("(k p) n -> p k n", p=128)
                            wd_v = moe_wd[0, r].rearrange("(j p) n -> p j n", p=128)
                            ffn_block(FB, hTe, hTe_b, capr, wg_v, wu_v, wd_v, NFE, groups, evacE, "me", state=fb_state)
                            for tl in range(nslr):
                                fw.dma(act, YS[o0 + tl * 128: o0 + (tl + 1) * 128, :], YACC[:, tl, :], d_ys[tl], reads=[yacc_b[tl]], writes=[ys_b[tl]])
                        fw.barrier()
                    fw.barrier()

                with ExitStack() as C:
                    yg = [[sb(C, f"yg{i}{k}", [128, D], F32) for k in range(2)] for i in range(2)]
                    yg_b = [[Buf(), Buf()], [Buf(), Buf()]]
                    acc = [sb(C, f"acc{i}", [128, D], F32) for i in range(2)]; acc_b = [Buf(), Buf()]
                    FG = sb(C, "FG", [128, D], F32); fg_b = Buf()
                    fw.dma(sp, FG[:], fgain.partition_broadcast(128), d_const, writes=[fg_b])
                    ssv = [sb(C, f"oss{i}", [128, 16], F32) for i in range(2)]; ss_b = [Buf(), Buf()]
                    jk = sb(C, "jk", [128, D], BF16); jk_b = Buf()
                    ho = [sb(C, f"ho{i}", [128, D], F32) for i in range(2)]; ho_b = [Buf(), Buf()]

                    def gath(ti):
                        i = ti % 2
                        for k in range(2):
                            act.emit(lambda e: e.memzero(yg[i][k][:]), writes=[yg_b[i][k]])
                            pool.deps(ys_b + [pos_b[ti]], [yg_b[i][k]])
                            pool.obj.indirect_dma_start(out=yg[i][k][:, :], out_offset=None, in_=YS[:, :],
                                                        in_offset=bass.IndirectOffsetOnAxis(ap=POS[:, ti, k:k + 1], axis=0),
                                                        bounds_check=bc_reg, oob_is_err=False).then_inc(d_g[i][k].sem, 16)
                            d_g[i][k].count += 16
                            tok = (d_g[i][k].sem, d_g[i][k].count)
                            fw.note(tok, [pos_b[ti]], [yg_b[i][k]])

                    def comb(ti):
                        t = ti + 1
                        i = ti % 2
                        act.emit(lambda e: e.activation(acc[i][:], yg[i][0][:], AF.Copy, scale=GATE[:, ti, 0:1]), reads=[yg_b[i][0], gate_b], writes=[acc_b[i]])
                        dve.emit(lambda e: e.scalar_tensor_tensor(acc[i][:], yg[i][1][:], GATE[:, ti, 1:2], acc[i][:], ALU.mult, ALU.add),
                                 reads=[yg_b[i][1], gate_b, acc_b[i]], writes=[acc_b[i]])
                        dve.emit(lambda e: e.tensor_tensor(acc[i][:], acc[i][:], G2V[:], ALU.mult), reads=[acc_b[i], g2v_b], writes=[acc_b[i]])
                        dve.emit(lambda e: e.tensor_tensor(XRES[:, t, :], XRES[:, t, :], acc[i][:], ALU.add), reads=[acc_b[i], xres_b[t]], writes=[xres_b[t]])

                    def fin_a(ti):
                        t = ti + 1
                        i = ti % 2
                        act.emit(lambda e: e.activation(jk[:], XRES[:, t, :], AF.Square, accum_out=ssv[i][:, 0:1]), reads=[xres_b[t]], writes=[jk_b, ss_b[i]])
                        dve.emit(lambda e: e.tensor_scalar(ssv[i][:, 0:1], ssv[i][:, 0:1], 1.0 / D, EPS, ALU.mult, ALU.add), reads=[ss_b[i]], writes=[ss_b[i]])
                        act.emit(lambda e: e.activation(ssv[i][:, 0:1], ssv[i][:, 0:1], AF.Sqrt), reads=[ss_b[i]], writes=[ss_b[i]])
                        dve.emit(lambda e: e.reciprocal(ssv[i][:, 0:1], ssv[i][:, 0:1]), reads=[ss_b[i]], writes=[ss_b[i]])
                        dve.emit(lambda e: e.scalar_tensor_tensor(ho[i][:], XRES[:, t, :], ssv[i][:, 0:1], FG[:], ALU.mult, ALU.mult), reads=[xres_b[t], ss_b[i], fg_b], writes=[ho_b[i]])
                        fw.dma(sp, out_d[ti * 128:(ti + 1) * 128, :], ho[i][:], d_out, reads=[ho_b[i]])

                    gath(0)
                    for it in range(17):
                        if it + 1 < 16:
                            gath(it + 1)
                        if it < 16:
                            comb(it)
                        if it - 1 >= 0:
                            fin_a(it - 1)
                    for d_ in d_out.sems:
                        if d_.count > 0:
                            sp.obj.wait_ge(d_.sem, d_.count)
                    fw.barrier()
                fw.barrier()

        if upto in ("ret", "ffn", "pool"):
            for i in range(16):
                fw.dma(sp, out_d[i * 128:(i + 1) * 128, :], XRES[:, 1 + i, :], d_out, reads=[xres_b[1 + i]])
            for d_ in d_out.sems:
                if d_.count > 0:
                    sp.obj.wait_ge(d_.sem, d_.count)
    return nc


def make_in_maps(inputs):
    x = np.ascontiguousarray(np.asarray(inputs["x"], dtype=np.float32)[0])
    pad = np.zeros(((NDEEP + 1) * 128, D), np.float32)
    xp = np.concatenate([pad, x], 0)
    consts = _const_tables()
    shared = {k: np.ascontiguousarray(np.asarray(v, dtype=np.float32)) for k, v in inputs.items() if k != "x"}
    maps = []
    for c in range(NCORES):
        m = dict(shared)
        m.update(consts)
        m.update(_host_tables(c))
        m["xext"] = np.ascontiguousarray(xp[c * TOWN: c * TOWN + NTILE * 128])
        maps.append(m)
    return maps


def kernel(**inputs):
    nc = build_program("all")
    maps = make_in_maps(inputs)
    res = run_bass_kernel_spmd(nc, maps, core_ids=list(range(NCORES)))
    out = np.concatenate([r["out"] for r in res.results], 0)
    return out.reshape(1, NCORES * TOWN, D).astype(np.float32)
```

```python
import numpy as np
from contextlib import ExitStack
import concourse.bass as bass
import concourse.mybir as mybir
from concourse.bass_utils import run_bass_kernel_spmd

F32 = mybir.dt.float32
BF16 = mybir.dt.bfloat16
U32 = mybir.dt.uint32
AF = mybir.ActivationFunctionType
ALU = mybir.AluOpType
AX = mybir.AxisListType

NCORES = 8
D = 1024
TOWN = 2048
NDEEP = 32
NTILE = NDEEP + 1 + 16
HALO = NDEEP
DFF = 2816
NF = DFF // 128
DFE = 3584
NFE = DFE // 128
EPS = 1e-6
CAPS = [1024] * 8
OFFS = [sum(CAPS[:r]) for r in range(8)]
TOTS = sum(CAPS)
CAPMAX = max(CAPS)
NSLMAX = CAPMAX // 128
GAMMA = [1.0 - 2.0 ** (-5.0 - h) for h in range(4)]

SAME_ENGINE_SYNC = True
RET_ORDER = 0
NPJ = 4
SEQ_HEADS = True
RET_W2 = 4
RET_W3 = 2


class Buf:
    __slots__ = ("ap", "lw", "rd", "name")

    def __init__(self, ap=None, name=""):
        self.ap = ap
        self.lw = None
        self.rd = {}
        self.name = name


class Eng:
    def __init__(self, fw, name, obj, sem):
        self.fw = fw
        self.name = name
        self.obj = obj
        self.sem = sem
        self.count = 0
        self.waited = {}
        self.is_pe = name == "pe"

    def _wait(self, tok):
        if tok is None:
            return
        key, val = tok
        if key is self.sem and (self.is_pe or not SAME_ENGINE_SYNC):
            return
        if self.waited.get(id(key), 0) >= val:
            return
        self.waited[id(key)] = val
        self.obj.wait_ge(key, val)

    def deps(self, reads, writes):
        for b in reads:
            self._wait(b.lw)
        for b in writes:
            self._wait(b.lw)
            for k, v in list(b.rd.items()):
                self._wait((self.fw.semobj[k], v))

    def emit(self, fn, reads=(), writes=(), signal=True):
        self.deps(reads, writes)
        ins = fn(self.obj)
        if signal:
            ins.then_inc(self.sem, 1)
            self.count += 1
            tok = (self.sem, self.count)
        else:
            tok = (self.sem, self.count + 1)
        self.fw.note(tok, reads, writes)
        return tok


class DSem:
    def __init__(self, sem):
        self.sem = sem
        self.count = 0


class DSemPool:
    def __init__(self, fw, name, n):
        self.sems = [fw.dsem(f"{name}{i}") for i in range(n)]
        self.i = 0

    def next(self, q):
        d = self.sems[self.i % len(self.sems)]
        self.i += 1
        if d.count > 0:
            q._wait((d.sem, d.count))
        return d


class FW:
    def __init__(self, nc, stack):
        self.nc = nc
        self.stack = stack
        self.semobj = {}
        self.dsems = []
        self.pe = self._mk("pe", nc.tensor)
        self.act = self._mk("act", nc.scalar)
        self.dve = self._mk("dve", nc.vector)
        self.pool = self._mk("pool", nc.gpsimd)
        self.sp = self._mk("sp", nc.sync)
        self.engines = [self.pe, self.act, self.dve, self.pool, self.sp]

    def _sem(self, name):
        s = self.stack.enter_context(self.nc.semaphore(name))
        self.semobj[id(s)] = s
        return s

    def _mk(self, name, obj):
        return Eng(self, name, obj, self._sem("s_" + name))

    def dsem(self, name):
        d = DSem(self._sem("d_" + name))
        self.dsems.append(d)
        return d

    def note(self, tok, reads, writes):
        k = id(tok[0])
        for b in reads:
            if b.rd.get(k, 0) < tok[1]:
                b.rd[k] = tok[1]
        for b in writes:
            b.lw = tok
            b.rd = {}

    def dma(self, q, out, in_, dsem, reads=(), writes=(), **kw):
        if isinstance(dsem, DSemPool):
            dsem = dsem.next(q)
        q.deps(reads, writes)
        q.obj.dma_start(out=out, in_=in_, **kw).then_inc(dsem.sem, 16)
        dsem.count += 16
        tok = (dsem.sem, dsem.count)
        self.note(tok, reads, writes)
        return tok

    def sync_on(self, toks):
        for e in self.engines:
            for t in toks:
                if t is not None:
                    e._wait(t)

    def barrier(self):
        for e in self.engines:
            for o in self.engines:
                if o is not e and o.count > 0:
                    e._wait((o.sem, o.count))
            for d in self.dsems:
                if d.count > 0:
                    e._wait((d.sem, d.count))


def _host_tables(core):
    start = core * TOWN
    pos = start - (NDEEP + 1) * 128 + np.arange(NTILE * 128)
    inv = (10000.0 ** (-np.arange(128, dtype=np.float32) / 128)).astype(np.float32)
    ang = (np.maximum(pos, 0).astype(np.float32)[:, None] * inv[None, :]).astype(np.float32)
    cs = np.concatenate([np.cos(ang.astype(np.float64)), np.sin(ang.astype(np.float64))], -1).astype(np.float32)
    valid = np.zeros((128, NTILE), np.float32)
    for j in range(NTILE):
        valid[:, j] = 1.0 if pos[j * 128] >= 0 else 0.0
    wins = (2, 4, 8, 16)
    bc = np.zeros((128, 4, 128), np.float32)
    bp = np.zeros((128, 4, 128), np.float32)
    bc0 = np.zeros((128, 4, 128), np.float32)
    for g, w in enumerate(wins):
        for t in range(128):
            for tp in range(t - w + 1, t + 1):
                if tp >= 0:
                    bc[tp, g, t] += 1.0 / w
                else:
                    bp[128 + tp, g, t] += 1.0 / w
            bc[t, g, t] -= 1.0
            gt = start + t
            cnt = min(gt + 1, w)
            for tp in range(t - w + 1, t + 1):
                if tp >= 0:
                    bc0[tp, g, t] += 1.0 / cnt
            bc0[t, g, t] -= 1.0
    if core > 0:
        bp0 = bp.copy()
        bc0 = bc.copy()
    else:
        bp0 = np.zeros_like(bp)
    return dict(cs=cs, valid=valid, pbc=bc.reshape(128, 512), pbp=bp.reshape(128, 512), pbc0=bc0.reshape(128, 512), pbp0=bp0.reshape(128, 512))


def _const_tables():
    c = np.arange(128, dtype=np.float64)
    mask = np.zeros((128, 4, 128), np.float32)
    qdb = np.zeros((128, 1024), np.float32)
    kdb = np.zeros((128, 1024), np.float32)
    for h in range(4):
        lg = np.log1p(-2.0 ** (-5.0 - h))
        mm = np.exp(-lg * (c + 1.0))[:, None] * (c[None, :] >= c[:, None]) / 16.0
        mask[:, h, :] = mm
        qdb[:, h * 256:(h + 1) * 256] = np.exp(lg * (c + 1.0))[:, None]
        kdb[:, h * 256:(h + 1) * 256] = (np.exp(lg * (127.0 - c)) / 16.0)[:, None]
    mt = np.zeros((128, 96), np.float32)
    mt[:, 0:8] = np.asarray(CAPS, np.float32)[None, :]
    mt[:, 8:16] = np.asarray(OFFS, np.float32)[None, :]
    mt[:, 16:24] = np.arange(8, dtype=np.float32)[None, :]
    lt = (np.arange(8)[None, :] < np.arange(8)[:, None]).astype(np.float32)
    mt[:, 32:96] = lt.reshape(1, 64)
    return dict(rmask=mask.reshape(128, 512), qdb=qdb, kdb=kdb, moetab=mt)


def build_program(upto="all"):
    nc = bass.Bass("TRN2", target_bir_lowering=False)

    def din(name, shape, dt=F32):
        return nc.dram_tensor(name, list(shape), dt, kind="ExternalInput").ap()

    xext = din("xext", [NTILE * 128, D])
    cs_d = din("cs", [NTILE * 128, 256])
    valid_d = din("valid", [128, NTILE])
    rmask_d = din("rmask", [128, 512])
    qdb_d = din("qdb", [128, 1024])
    kdb_d = din("kdb", [128, 1024])
    pbc_d = din("pbc", [128, 512])
    pbp_d = din("pbp", [128, 512])
    pbc0_d = din("pbc0", [128, 512])
    pbp0_d = din("pbp0", [128, 512])
    moetab_d = din("moetab", [128, 96])
    c_d = din("c", [1, D])
    ada_w = din("ada_w", [2, D, 6 * D])
    ada_b = din("ada_b", [2, 6 * D])
    norm_gain = din("norm_gain", [2, 2, D])
    ret_w_in = din("ret_w_in", [1, D, 6144])
    ret_gn = din("ret_gn_gain", [1, 2048])
    ret_w_out = din("ret_w_out", [1, 2048, D])
    ffn_wg = din("ffn_w_gate", [1, D, DFF])
    ffn_wu = din("ffn_w_up", [1, D, DFF])
    ffn_wd = din("ffn_w_down", [1, DFF, D])
    pool_w = din("pool_w", [1, 4, 256, 256])
    pool_b = din("pool_b", [1, 4, 256])
    pool_scale = din("pool_scale", [1, D])
    moe_router = din("moe_router", [1, D, 8])
    moe_wg = din("moe_w_gate", [1, 8, D, DFE])
    moe_wu = din("moe_w_up", [1, 8, D, DFE])
    moe_wd = din("moe_w_down", [1, 8, DFE, D])
    fgain = din("final_norm_gain", [D])
    out_d = nc.dram_tensor("out", [TOWN, D], F32, kind="ExternalOutput").ap()

    with ExitStack() as top:
        fw = FW(nc, top)
        pe, act, dve, pool, sp = fw.pe, fw.act, fw.dve, fw.pool, fw.sp

        uid = [0]

        def sb(stack, name, shape, dt):
            uid[0] += 1
            return stack.enter_context(nc.sbuf_tensor(f"{name}_{uid[0]}", list(shape), dt))

        def ps(stack, name, shape, dt):
            uid[0] += 1
            return stack.enter_context(nc.psum_tensor(f"{name}_{uid[0]}", list(shape), dt))

        XRES = sb(top, "XRES", [128, 17, D], F32)
        xres_b = [Buf(name=f"xres{i}") for i in range(17)]
        ident = sb(top, "ident", [128, 128], BF16); ident_b = Buf()
        ones_row = sb(top, "ones_row", [1, 128], F32); ones_b = Buf()
        cb = sb(top, "cb", [128, 8, 128], F32); cb_b = Buf()
        eps_t = sb(top, "eps_t", [128, 1], F32)
        d_const = DSemPool(fw, "const", 8)
        d_x = [fw.dsem("x0"), fw.dsem("x1")]
        d_w = [fw.dsem(f"w{i}") for i in range(4)]
        d_out = DSemPool(fw, "out", 2)
        d_win = fw.dsem("win")
        d_pb = [fw.dsem(f"pb{i}") for i in range(4)]

        pool.emit(lambda e: e.memset(ident[:], 1.0), writes=[ident_b])
        pool.emit(lambda e: e.affine_select(ident[:], ident[:], [[-1, 128]], ALU.is_equal, 0.0, base=0, channel_multiplier=1),
                  reads=[ident_b], writes=[ident_b])
        pool.emit(lambda e: e.memset(ones_row[:], 1.0), writes=[ones_b])
        eps_b = Buf()
        pool.emit(lambda e: e.memset(eps_t[:], EPS), writes=[eps_b])
        HS = nc.dram_tensor("hs_scratch", [TOTS, D], BF16)
        YS = nc.dram_tensor("y_scratch", [TOTS, D], F32)
        hs_b = Buf()
        d_hs = fw.dsem("hs")
        if upto == "all":
            ZT = sb(top, "ZT", [128, D], BF16); zt_b = Buf()
            pool.emit(lambda e: e.memset(ZT[:], 0.0), writes=[zt_b])
            for n in range(TOTS // 128):
                fw.dma(pool, HS[n * 128:(n + 1) * 128, :], ZT[:], d_hs, reads=[zt_b], writes=[hs_b])
                hs_b.lw = None
            hs_b.lw = (d_hs.sem, d_hs.count)

        with ExitStack() as st:
            crow = sb(st, "crow", [1, D], F32); crow_b = Buf()
            pcb = ps(st, "pcb", [128, 8, 128], F32); pcb_b = Buf()
            fw.dma(sp, crow[:], c_d[:, :], d_const, writes=[crow_b])
            act.emit(lambda e: e.activation(crow[:], crow[:], AF.Silu), reads=[crow_b], writes=[crow_b])
            for kc in range(8):
                pe.emit(lambda e: e.matmul(pcb[:, kc, :], crow[:, kc * 128:(kc + 1) * 128], ones_row[:], start=True, stop=True),
                        reads=[crow_b, ones_b], writes=[pcb_b], signal=(kc == 7))
            dve.emit(lambda e: e.tensor_copy(cb[:], pcb[:]), reads=[pcb_b], writes=[cb_b])
            fw.barrier()

        def compute_mod(layer, half, MODB, modb_b):
            with ExitStack() as st:
                awt = [sb(st, f"awt{i}", [128, 8, 512], F32) for i in range(2)]
                awt_b = [Buf(), Buf()]
                brow = [sb(st, f"brow{i}", [1, 512], F32) for i in range(2)]
                brow_b = [Buf(), Buf()]
                gbc = sb(st, "gbc", [128, D], F32); gbc_b = Buf()
                pm = [ps(st, f"pmod{i}", [128, 512], F32) for i in range(2)]
                pm_b = [Buf(), Buf()]
                tok = fw.dma(sp, gbc[:], norm_gain[layer, half, :].partition_broadcast(128), d_const, writes=[gbc_b])
                for b in range(6):
                    col = half * 3072 + b * 512
                    i = b % 2
                    fw.dma(sp, awt[i][:], ada_w[layer, :, col:col + 512].rearrange("(k p) n -> p k n", p=128), d_w[i], writes=[awt_b[i]])
                    fw.dma(sp, brow[i][:], ada_b[layer:layer + 1, col:col + 512], d_w[2 + i], writes=[brow_b[i]])
                    for kc in range(8):
                        pe.emit(lambda e: e.matmul(pm[i][:], cb[:, kc, :], awt[i][:, kc, :], start=(kc == 0), stop=False),
                                reads=[cb_b, awt_b[i]], writes=[pm_b[i]], signal=False)
                    pe.emit(lambda e: e.matmul(pm[i][:], ones_row[:], brow[i][:], start=False, stop=True),
                            reads=[ones_b, brow_b[i]], writes=[pm_b[i]])
                    act.emit(lambda e: e.copy(MODB[:, b // 2, (b % 2) * 512:(b % 2) * 512 + 512], pm[i][:]), reads=[pm_b[i]], writes=[modb_b])
                dve.emit(lambda e: e.scalar_tensor_tensor(MODB[:, 1, :], MODB[:, 1, :], 1.0, gbc[:], ALU.add, ALU.mult),
                         reads=[modb_b, gbc_b], writes=[modb_b])
                fw.barrier()

        MODS = nc.dram_tensor("mods_scratch", [4, 3072], F32)
        mods_b = [[Buf(), Buf()] for _ in range(4)]
        d_mods = [fw.dsem("mods0"), fw.dsem("mods1")]

        def mod_rows_gen(stack, items):
            NA = 4
            awt = [sb(stack, f"bawt{i}", [128, 8, 512], F32) for i in range(NA)]; awt_b = [Buf() for _ in range(NA)]
            brow = [sb(stack, f"bbrow{i}", [1, 512], F32) for i in range(NA)]; brow_b = [Buf() for _ in range(NA)]
            rowo = [sb(stack, f"browo{i}", [1, 512], F32) for i in range(2)]; rowo_b = [Buf(), Buf()]
            pm = [ps(stack, f"bpmod{i}", [128, 512], F32) for i in range(2)]; pm_b = [Buf(), Buf()]
            d_bw = [fw.dsem(f"bw{i}") for i in range(NA)]
            blocks = [(layer, half, b) for (layer, half) in items for b in range(6)]

            def issue(n):
                layer, half, b = blocks[n]
                col = half * 3072 + b * 512
                a = n % NA
                fw.dma(sp, awt[a][:], ada_w[layer, :, col:col + 512].rearrange("(k p) n -> p k n", p=128), d_bw[a], writes=[awt_b[a]])
                awt_b[a].lw = None
                fw.dma(sp, brow[a][:], ada_b[layer:layer + 1, col:col + 512], d_bw[a], writes=[brow_b[a]])
                awt_b[a].lw = brow_b[a].lw

            for n in range(min(NA - 1, len(blocks))):
                issue(n)
            for n, (layer, half, b) in enumerate(blocks):
                idx = layer * 2 + half
                a = n % NA
                i = n % 2
                if n + NA - 1 < len(blocks):
                    issue(n + NA - 1)
                if True:
                    yield
                    for kc in range(8):
                        pe.emit(lambda e: e.matmul(pm[i][:], cb[:, kc, :], awt[a][:, kc, :], start=(kc == 0), stop=False),
                                reads=[cb_b, awt_b[a]], writes=[pm_b[i]], signal=False)
                    pe.emit(lambda e: e.matmul(pm[i][:], ones_row[:], brow[a][:], start=False, stop=True),
                            reads=[ones_b, brow_b[a]], writes=[pm_b[i]])
                    yield
                    act.emit(lambda e: e.copy(rowo[i][:], pm[i][0:1, :]), reads=[pm_b[i]], writes=[rowo_b[i]])
                    fw.dma(sp, MODS[idx:idx + 1, b * 512:(b + 1) * 512], rowo[i][:], d_mods[i], reads=[rowo_b[i]], writes=[mods_b[idx][i]])
                    yield

        def load_mod(layer, half, MODB, modb_b, stack):
            idx = layer * 2 + half
            gbc = sb(stack, "gbc", [128, D], F32); gbc_b = Buf()
            fw.dma(sp, gbc[:], norm_gain[layer, half, :].partition_broadcast(128), d_const, writes=[gbc_b])
            fw.dma(sp, MODB[:].rearrange("p s d -> p (s d)"), MODS[idx, :].partition_broadcast(128), d_const, reads=mods_b[idx], writes=[modb_b])
            dve.emit(lambda e: e.scalar_tensor_tensor(MODB[:, 1, :], MODB[:, 1, :], 1.0, gbc[:], ALU.add, ALU.mult),
                     reads=[modb_b, gbc_b], writes=[modb_b])

        def emit_norm(xap, x_b, G, SH, g_b, junk, junk_b, ss, ss_b, hn, hn_b, h16, h16_b):
            act.emit(lambda e: e.activation(junk[:], xap, AF.Square, accum_out=ss[:]), reads=[x_b], writes=[junk_b, ss_b])
            dve.emit(lambda e: e.tensor_scalar(ss[:], ss[:], 1.0 / D, EPS, ALU.mult, ALU.add), reads=[ss_b], writes=[ss_b])
            act.emit(lambda e: e.activation(ss[:], ss[:], AF.Sqrt), reads=[ss_b], writes=[ss_b])
            dve.emit(lambda e: e.reciprocal(ss[:], ss[:]), reads=[ss_b], writes=[ss_b])
            dve.emit(lambda e: e.scalar_tensor_tensor(hn[:], xap, ss[:], G, ALU.mult, ALU.mult), reads=[x_b, ss_b, g_b], writes=[hn_b])
            if SH is not None:
                pool.emit(lambda e: e.tensor_tensor(h16[:], hn[:], SH, ALU.add), reads=[hn_b, g_b], writes=[h16_b])

        def interleave(gens, weights):
            gens = list(gens)
            alive = [True] * len(gens)
            while any(alive):
                for gi, g in enumerate(gens):
                    if not alive[gi]:
                        continue
                    for _ in range(weights[gi]):
                        try:
                            next(g)
                        except StopIteration:
                            alive[gi] = False
                            break

        HT0 = nc.dram_tensor("ht_scratch", [NTILE, 128, 8, 128], BF16)
        ht_b = [Buf() for _ in range(NTILE)]
        d_ht = DSemPool(fw, "ht", 4)
        with ExitStack() as L0:
            G1V = sb(L0, "G1V", [128, D], F32); g1v_b = Buf()
            rmask = sb(L0, "rmask", [128, 4, 128], F32); rmask_b = Buf()
            VAL = sb(L0, "VAL", [128, NTILE], F32); val_b = Buf()
            fw.dma(sp, rmask[:], rmask_d.rearrange("p (h c) -> p h c", h=4), d_const, writes=[rmask_b])
            fw.dma(sp, VAL[:], valid_d[:, :], d_const, writes=[val_b])
            with ExitStack() as PP0:
                MODA = sb(PP0, "MODA", [128, 3, D], F32); moda_b = Buf()
                compute_mod(0, 0, MODA, moda_b)
                dve.emit(lambda e: e.tensor_copy(G1V[:], MODA[:, 2, :]), reads=[moda_b], writes=[g1v_b])
                NB = 3
                xt = [sb(PP0, f"pxt{i}", [128, D], F32) for i in range(NB)]; xt_b = [Buf() for _ in range(NB)]
                sq = [sb(PP0, f"psq{i}", [128, D], BF16) for i in range(2)]; sq_b = [Buf(), Buf()]
                ssv = [sb(PP0, f"pss{i}", [128, 1], F32) for i in range(NB)]; ss_b = [Buf() for _ in range(NB)]
                hn = [sb(PP0, f"phn{i}", [128, D], F32) for i in range(2)]; hn_b = [Buf(), Buf()]
                h16 = [sb(PP0, f"ph16{i}", [128, D], BF16) for i in range(2)]; h16_b = [Buf(), Buf()]
                hTo = [sb(PP0, f"phT{i}", [128, 8, 128], BF16) for i in range(2)]; hTo_b = [Buf(), Buf()]
                PTP = [ps(PP0, f"PTP{i}", [128, 8, 128], BF16) for i in range(2)]; PTP_b = [Buf(), Buf()]

                def pp_ld(j):
                    i = j % NB
                    fw.dma(sp, xt[i][:], xext[j * 128:(j + 1) * 128, :], d_xp[i], writes=[xt_b[i]])

                def pp_s0(j):
                    i = j % NB
                    if j + 1 < NTILE:
                        pp_ld(j + 1)
                    act.emit(lambda e: e.activation(sq[j % 2][:], xt[i][:], AF.Square, accum_out=ssv[i][:]), reads=[xt_b[i]], writes=[sq_b[j % 2], ss_b[i]])
                    yield

                def pp_s1(j):
                    i = j % NB
                    k = j % 2
                    dve.emit(lambda e: e.tensor_scalar(ssv[i][:], ssv[i][:], 1.0 / D, EPS, ALU.mult, ALU.add), reads=[ss_b[i]], writes=[ss_b[i]])
                    act.emit(lambda e: e.activation(ssv[i][:], ssv[i][:], AF.Sqrt), reads=[ss_b[i]], writes=[ss_b[i]])
                    yield
                    dve.emit(lambda e: e.reciprocal(ssv[i][:], ssv[i][:]), reads=[ss_b[i]], writes=[ss_b[i]])
                    dve.emit(lambda e: e.scalar_tensor_tensor(hn[k][:], xt[i][:], ssv[i][:], MODA[:, 1, :], ALU.mult, ALU.mult), reads=[xt_b[i], ss_b[i], moda_b], writes=[hn_b[k]])
                    (pool if j % 2 == 0 else dve).emit(lambda e: e.tensor_tensor(h16[k][:], hn[k][:], MODA[:, 0, :], ALU.add), reads=[hn_b[k], moda_b], writes=[h16_b[k]])
                    yield

                def pp_s2(j):
                    k = j % 2
                    for kc in range(8):
                        pe.emit(lambda e: e.transpose(PTP[k][:, kc, :], h16[k][:, kc * 128:(kc + 1) * 128], ident[:]),
                                reads=[h16_b[k], ident_b], writes=[PTP_b[k]], signal=(kc == 7))
                    act.emit(lambda e: e.copy(hTo[k][:], PTP[k][:]), reads=[PTP_b[k]], writes=[hTo_b[k]])
                    fw.dma(act, HT0[j], hTo[k][:], d_ht, reads=[hTo_b[k]], writes=[ht_b[j]])
                    yield

                d_xp = [fw.dsem(f"xp{i}") for i in range(NB)]
                pp_ld(0)
                bg = mod_rows_gen(PP0, [(0, 1), (1, 0), (1, 1)])
                bg_alive = True
                for it in range(NTILE + 2):
                    gens = []
                    if 0 <= it - 1 < NTILE:
                        gens.append(pp_s1(it - 1))
                    if it < NTILE:
                        gens.append(pp_s0(it))
                    if it - 2 >= 0:
                        gens.append(pp_s2(it - 2))
                    for g_ in gens:
                        for _ in g_:
                            pass
                    if bg_alive:
                        for _ in range(2):
                            try:
                                next(bg)
                            except StopIteration:
                                bg_alive = False
                                break
                while bg_alive:
                    try:
                        next(bg)
                    except StopIteration:
                        bg_alive = False
                fw.barrier()

            for grp in range(2):
                with ExitStack() as G:
                    WIN = sb(G, "WIN", [128, 8, 3072], BF16); win_seg_b = [Buf() for _ in range(4)]
                    WOUT = sb(G, "WOUT", [128, 8, D], BF16); wout_b = Buf()
                    QDB = sb(G, "QDB", [128, 512], F32); qdb_b = Buf()
                    KDB = sb(G, "KDB", [128, 512], F32); kdb_b = Buf()
                    S32 = sb(G, "S32", [128, 2, 2, 512], F32); s32_b = [[Buf(), Buf()], [Buf(), Buf()]]
                    S16 = sb(G, "S16", [128, 2, 2, 512], BF16); s16_b = [[Buf(), Buf()], [Buf(), Buf()]]
                    wv = ret_w_in[0].rearrange("(k p) n -> p k n", p=128)
                    segs = [(1024 + grp * 512, 512, 512), (2048 + grp * 1024, 1024, 1024), (grp * 512, 512, 0), (4096 + grp * 1024, 1024, 2048)]
                    d_wins = [d_win, d_pb[0], d_pb[1], d_pb[2]]
                    for si, (src, n, dst) in enumerate(segs):
                        fw.dma(pool, WIN[:, :, dst:dst + n], wv[:, :, src:src + n], d_wins[si], writes=[win_seg_b[si]])

                    def win_buf(col0):
                        if col0 < 512:
                            return win_seg_b[2]
                        if col0 < 1024:
                            return win_seg_b[0]
                        if col0 < 2048:
                            return win_seg_b[1]
                        return win_seg_b[3]
                    fw.dma(sp, QDB[:], qdb_d[:, grp * 512:(grp + 1) * 512], d_const, writes=[qdb_b])
                    fw.dma(sp, KDB[:], kdb_d[:, grp * 512:(grp + 1) * 512], d_const, writes=[kdb_b])
                    with ExitStack() as st:
                        WST = sb(st, "WST", [128, 4, D], F32); wst_b = Buf()
                        gn = sb(st, "gn", [128, 8], F32); gn_b = Buf()
                        fw.dma(sp, gn[:], ret_gn[0, grp * 1024:(grp + 1) * 1024].rearrange("(k p) -> p k", p=128), d_const,
                               writes=[gn_b], allow_slow_non_contiguous=True)
                        wo = ret_w_out[0, grp * 1024:(grp + 1) * 1024, :].rearrange("(k p) n -> p k n", p=128)
                        for hf in range(2):
                            fw.dma(sp, WST[:], wo[:, hf * 4:(hf + 1) * 4, :], d_w[1], writes=[wst_b])
                            for k4 in range(4):
                                kc = hf * 4 + k4
                                dve.emit(lambda e: e.scalar_tensor_tensor(WOUT[:, kc, :], WST[:, k4, :], gn[:, kc:kc + 1], G1V[:], ALU.mult, ALU.mult),
                                         reads=[wst_b, gn_b, g1v_b], writes=[wout_b])
                        fw.sync_on([(dve.sem, dve.count)])
                    dve.emit(lambda e: e.memset(S32[:], 0.0), writes=s32_b[0] + s32_b[1])
                    pool.emit(lambda e: e.memset(S16[:], 0.0), writes=s16_b[0] + s16_b[1])

                    with ExitStack() as T:
                        hTt = [sb(T, f"hTt{i}", [128, 8, 128], BF16) for i in range(2)]; hTt_b = [Buf(), Buf()]
                        xt = [sb(T, f"xt{i}", [128, D], F32) for i in range(2 if grp == 0 else 0)]; xt_b = [Buf(), Buf()]
                        cst = [sb(T, f"cst{i}", [128, 256], F32) for i in range(2)]; cst_b = [Buf(), Buf()]
                        tmp = [sb(T, f"rt{i}", [128, 2, 128], F32) for i in range(4)]; tmp_b = [Buf() for _ in range(4)]
                        rotq = sb(T, "rotq", [128, 2, 256], BF16); rotq_b = Buf()
                        rotk = [sb(T, f"rotk{i}", [128, 2, 256], BF16) for i in range(2)]; rotk_b = [Buf(), Buf()]
                        qd = sb(T, "qd", [128, 512], BF16); qd_b = Buf()
                        ktok = [sb(T, f"ktok{i}", [128, 512], BF16) for i in range(2)]; ktok_b = [Buf(), Buf()]
                        qkT = [sb(T, f"qkT{i}", [128, 8, 128], BF16) for i in range(2)]; qkT_b = [Buf(), Buf()]
                        v16 = [sb(T, f"v16{i}", [128, 1024], BF16) for i in range(2)]; v16_b = [[Buf(), Buf()], [Buf(), Buf()]]
                        sg = [sb(T, f"sg{i}", [128, 1024], BF16) for i in range(2)]; sg_b = [[Buf(), Buf()], [Buf(), Buf()]]
                        sT16 = sb(T, "sT16", [128, 2, 128], BF16); sT_b = [Buf(), Buf()]
                        on32_0 = sb(T, "on32", [128, 512], F32); on32_0b = Buf()
                        on32 = [on32_0, on32_0]; on_b = [on32_0b, on32_0b]
                        y16s = [sb(T, f"y16_{i}", [128, 1024], BF16) for i in range(2)]; y16s_b = [[Buf(), Buf()], [Buf(), Buf()]]
                        yT = sb(T, "yT", [128, 8, 128], BF16); yT_b = Buf()
                        bst_t = [sb(T, f"bst{i}", [128, 32], F32) for i in range(2)]; bst_b = [Buf(), Buf()]
                        mv_t = [sb(T, f"mv{i}", [128, 32], F32) for i in range(2)]; mv_b = [Buf(), Buf()]
                        bst = [t_[:, 0:6] for t_ in bst_t]
                        mv = [t_[:, 0:2] for t_ in mv_t]
                        PJ = [ps(T, f"PJ{i}", [128, 512], F32) for i in range(NPJ)]; PJ_b = [Buf() for _ in range(NPJ)]
                        PTA = ps(T, "PTA", [128, 8, 128], BF16); PTA_b = Buf()
                        PTY = ps(T, "PTY", [128, 8, 128], BF16); PTY_b = Buf()
                        PSC = ps(T, "PSC", [128, 2, 128], F32); PSC_b = [Buf(), Buf()]
                        if NPJ == 4:
                            PO0 = ps(T, "PO0", [128, 512], F32); PO0_b = Buf()
                            PO = [PO0, PO0]; PO_b = [PO0_b, PO0_b]
                        else:
                            PO = [ps(T, f"PO{i}", [128, 512], F32) for i in range(2)]; PO_b = [Buf(), Buf()]
                        pj_ctr = [0]
                        d_xr = [fw.dsem(f"xr{grp}{i}") for i in range(2)]

                        def next_pj():
                            i = pj_ctr[0] % NPJ
                            pj_ctr[0] += 1
                            return PJ[i], PJ_b[i]

                        def proj(hT_, hT_bb, col0, ncol):
                            P, P_b = next_pj()
                            for kc in range(8):
                                pe.emit(lambda e: e.matmul(P[:, 0:ncol], hT_[:, kc, :], WIN[:, kc, col0:col0 + ncol], start=(kc == 0), stop=(kc == 7)),
                                        reads=[hT_bb, win_buf(col0)], writes=[P_b], signal=(kc == 7))
                            return P, P_b

                        def rotary(P, P_b, cs_t, cs_b, rot, rot_b, nh=2, h0=0):
                            X = P[:, 0:nh * 256].rearrange("p (h t i) -> p h t i", h=nh, t=2)
                            X1 = X[:, :, 0, :]
                            X2 = X[:, :, 1, :]
                            R = rot[:, h0:h0 + nh, :]
                            t_ = [tm[:, 0:nh, :] for tm in tmp]
                            cosb = cs_t[:, 0:128].unsqueeze(1).to_broadcast([128, nh, 128])
                            sinb = cs_t[:, 128:256].unsqueeze(1).to_broadcast([128, nh, 128])
                            dve.emit(lambda e: e.tensor_tensor(t_[0], X1, cosb, ALU.mult), reads=[P_b, cs_b], writes=[tmp_b[0]])
                            dve.emit(lambda e: e.tensor_tensor(t_[1], X2, sinb, ALU.mult), reads=[P_b, cs_b], writes=[tmp_b[1]])
                            pool.emit(lambda e: e.tensor_tensor(R[:, :, 0:128], t_[0], t_[1], ALU.subtract), reads=[tmp_b[0], tmp_b[1]], writes=[rot_b])
                            yield
                            dve.emit(lambda e: e.tensor_tensor(t_[2], X1, sinb, ALU.mult), reads=[P_b, cs_b], writes=[tmp_b[2]])
                            dve.emit(lambda e: e.tensor_tensor(t_[3], X2, cosb, ALU.mult), reads=[P_b, cs_b], writes=[tmp_b[3]])
                            pool.emit(lambda e: e.tensor_tensor(R[:, :, 128:256], t_[2], t_[3], ALU.add), reads=[tmp_b[2], tmp_b[3]], writes=[rot_b])
                            yield

                        def heads_of(j):
                            if grp == 1 and j < 16:
                                return [1]
                            if grp == 0 and j < NDEEP - 4:
                                return [1]
                            return [0, 1]

                        first_deep = (NDEEP - 8) if grp == 0 else 0

                        def stage2(j):
                            full = j >= HALO
                            bi = j % 2
                            fw.dma(sp, hTt[bi][:], HT0[j], d_x[bi], reads=[ht_b[j]], writes=[hTt_b[bi]])
                            hTt_b[bi].lw = None
                            fw.dma(sp, cst[bi][:], cs_d[j * 128:(j + 1) * 128, :], d_x[bi], writes=[cst_b[bi]])
                            hTt_b[bi].lw = cst_b[bi].lw
                            yield
                            hds = heads_of(j)
                            if len(hds) == 2:
                                Pk, Pk_b = proj(hTt[bi], hTt_b[bi], 512, 512)
                                yield
                                yield from rotary(Pk, Pk_b, cst[bi], cst_b[bi], rotk[bi], rotk_b[bi])
                                dve.emit(lambda e: e.scalar_tensor_tensor(ktok[bi][:], rotk[bi][:].rearrange("p h d -> p (h d)"), VAL[:, j:j + 1], KDB[:], ALU.mult, ALU.mult),
                                         reads=[rotk_b[bi], val_b, kdb_b], writes=[ktok_b[bi]])
                            else:
                                Pk, Pk_b = proj(hTt[bi], hTt_b[bi], 512 + 256, 256)
                                yield
                                yield from rotary(Pk, Pk_b, cst[bi], cst_b[bi], rotk[bi], rotk_b[bi], nh=1, h0=1)
                                dve.emit(lambda e: e.scalar_tensor_tensor(ktok[bi][:, 256:512], rotk[bi][:, 1, :], VAL[:, j:j + 1], KDB[:, 256:512], ALU.mult, ALU.mult),
                                         reads=[rotk_b[bi], val_b, kdb_b], writes=[ktok_b[bi]])
                            yield
                            for hh in hds:
                                Pv, Pv_b = proj(hTt[bi], hTt_b[bi], 1024 + hh * 512, 512)
                                act.emit(lambda e: e.copy(v16[bi][:, hh * 512:(hh + 1) * 512], Pv[:]), reads=[Pv_b], writes=[v16_b[bi][hh]])
                                yield
                            if full:
                                Pq, Pq_b = proj(hTt[bi], hTt_b[bi], 0, 512)
                                yield
                                yield from rotary(Pq, Pq_b, cst[bi], cst_b[bi], rotq, rotq_b)
                                pool.emit(lambda e: e.tensor_tensor(qd[:], rotq[:].rearrange("p h d -> p (h d)"), QDB[:], ALU.mult),
                                          reads=[rotq_b, qdb_b], writes=[qd_b])
                                yield
                                for hh in range(2):
                                    Pg, Pg_b = proj(hTt[bi], hTt_b[bi], 2048 + hh * 512, 512)
                                    act.emit(lambda e: e.activation(sg[bi][:, hh * 512:(hh + 1) * 512], Pg[:], AF.Silu), reads=[Pg_b], writes=[sg_b[bi][hh]])
                                    yield
                                rk = rotk[bi][:].rearrange("p h d -> p (h d)")
                                for blk in range(4):
                                    pe.emit(lambda e: e.transpose(PTA[:, blk, :], qd[:, blk * 128:(blk + 1) * 128], ident[:]),
                                            reads=[qd_b, ident_b], writes=[PTA_b], signal=False)
                                for blk in range(4):
                                    pe.emit(lambda e: e.transpose(PTA[:, 4 + blk, :], rk[:, blk * 128:(blk + 1) * 128], ident[:]),
                                            reads=[rotk_b[bi], ident_b], writes=[PTA_b], signal=(blk == 3))
                                act.emit(lambda e: e.copy(qkT[bi][:], PTA[:]), reads=[PTA_b], writes=[qkT_b[bi]])
                                yield

                        def head_chain(j, hh):
                            full = j >= HALO
                            bi = j % 2
                            h = grp * 2 + hh
                            if full:
                                for half in range(2):
                                    pe.emit(lambda e: e.matmul(PSC[:, hh, :], qkT[bi][:, 4 + hh * 2 + half, :], qkT[bi][:, hh * 2 + half, :], start=(half == 0), stop=(half == 1)),
                                            reads=[qkT_b[bi]], writes=[PSC_b[0], PSC_b[1]], signal=(half == 1))
                                dve.emit(lambda e: e.tensor_tensor(sT16[:, hh, :], PSC[:, hh, :], rmask[:, h, :], ALU.mult),
                                         reads=[PSC_b[hh], rmask_b], writes=[sT_b[hh]])
                                yield
                                pe.emit(lambda e: e.matmul(PO[hh][:], sT16[:, hh, :], v16[bi][:, hh * 512:(hh + 1) * 512], start=True, stop=False),
                                        reads=[sT_b[hh], v16_b[bi][hh]], writes=[PO_b[hh]], signal=False)
                                for half in range(2):
                                    pe.emit(lambda e: e.matmul(PO[hh][:], qkT[bi][:, hh * 2 + half, :], S16[:, hh, half, :], start=False, stop=(half == 1)),
                                            reads=[qkT_b[bi], s16_b[hh][half]], writes=[PO_b[hh]], signal=(half == 1))
                                dve.emit(lambda e: e.bn_stats(bst[hh], PO[hh][:]), reads=[PO_b[hh]], writes=[bst_b[hh]])
                                dve.emit(lambda e: e.bn_aggr(mv[hh], bst[hh]), reads=[bst_b[hh]], writes=[mv_b[hh]])
                                yield
                                act.emit(lambda e: e.activation(mv[hh][:, 1:2], mv[hh][:, 1:2], AF.Sqrt, bias=eps_t[:]), reads=[mv_b[hh], eps_b], writes=[mv_b[hh]])
                                dve.emit(lambda e: e.reciprocal(mv[hh][:, 1:2], mv[hh][:, 1:2]), reads=[mv_b[hh]], writes=[mv_b[hh]])
                                dve.emit(lambda e: e.tensor_scalar(on32[hh][:], PO[hh][:], mv[hh][:, 0:1], mv[hh][:, 1:2], ALU.subtract, ALU.mult),
                                         reads=[PO_b[hh], mv_b[hh]], writes=[on_b[hh]])
                                pool.emit(lambda e: e.tensor_tensor(y16s[bi][:, hh * 512:(hh + 1) * 512], on32[hh][:], sg[bi][:, hh * 512:(hh + 1) * 512], ALU.mult),
                                          reads=[on_b[hh], sg_b[bi][hh]], writes=[y16s_b[bi][hh]])
                                yield
                            gC = float(GAMMA[h] ** 128)
                            for half in range(2):
                                Pkv, Pkv_b = next_pj()
                                pe.emit(lambda e: e.matmul(Pkv[:], ktok[bi][:, hh * 256 + half * 128: hh * 256 + (half + 1) * 128], v16[bi][:, hh * 512:(hh + 1) * 512], start=True, stop=True),
                                        reads=[ktok_b[bi], v16_b[bi][hh]], writes=[Pkv_b])
                                dve.emit(lambda e: e.scalar_tensor_tensor(S32[:, hh, half, :], S32[:, hh, half, :], gC, Pkv[:], ALU.mult, ALU.add),
                                         reads=[Pkv_b, s32_b[hh][half]], writes=[s32_b[hh][half]])
                                if j < NTILE - 1:
                                    act.emit(lambda e: e.copy(S16[:, hh, half, :], S32[:, hh, half, :]), reads=[s32_b[hh][half]], writes=[s16_b[hh][half]])
                                yield

                        def stage3(j):
                            full = j >= HALO
                            bi = j % 2
                            if full and grp == 0:
                                fw.dma(sp, xt[bi][:], xext[j * 128:(j + 1) * 128, :], d_xr[bi], writes=[xt_b[bi]])
                            g0 = head_chain(j, 0)
                            g1 = head_chain(j, 1)
                            if len(heads_of(j)) == 1:
                                for _ in g1:
                                    yield
                            elif SEQ_HEADS:
                                for _ in g0:
                                    yield
                                for _ in g1:
                                    yield
                            else:
                                alive = [True, True]
                                if full:
                                    next(g0)
                                    yield
                                while any(alive):
                                    for gi, g in enumerate((g0, g1)):
                                        if alive[gi]:
                                            try:
                                                next(g)
                                            except StopIteration:
                                                alive[gi] = False
                                    yield
                        def stage4(j):
                            full = j >= HALO
                            bi = j % 2
                            if full:
                                ti = j - HALO
                                for kc in range(8):
                                    pe.emit(lambda e: e.transpose(PTY[:, kc, :], y16s[bi][:, kc * 128:(kc + 1) * 128], ident[:]),
                                            reads=[y16s_b[bi][kc // 4], ident_b], writes=[PTY_b], signal=(kc == 7))
                                act.emit(lambda e: e.copy(yT[:], PTY[:]), reads=[PTY_b], writes=[yT_b])
                                yield
                                for ch in range(2):
                                    Pm, Pm_b = next_pj()
                                    for kc in range(8):
                                        pe.emit(lambda e: e.matmul(Pm[:], yT[:, kc, :], WOUT[:, kc, ch * 512:(ch + 1) * 512], start=(kc == 0), stop=(kc == 7)),
                                                reads=[yT_b, wout_b], writes=[Pm_b], signal=(kc == 7))
                                    xr = XRES[:, ti, ch * 512:(ch + 1) * 512]
                                    if grp == 0:
                                        dve.emit(lambda e: e.tensor_tensor(xr, Pm[:], xt[bi][:, ch * 512:(ch + 1) * 512], ALU.add),
                                                 reads=[Pm_b, xt_b[bi]], writes=[xres_b[ti]])
                                    else:
                                        dve.emit(lambda e: e.tensor_tensor(xr, Pm[:], xr, ALU.add), reads=[Pm_b, xres_b[ti]], writes=[xres_b[ti]])
                                    yield

                            return
                            yield

                        steps = list(range(first_deep, NTILE))
                        for it in range(len(steps) + 2):
                            gens = []
                            wts = []
                            if it - 2 >= 0:
                                gens.append(stage4(steps[it - 2])); wts.append(1)
                            if RET_ORDER == 0:
                                if 0 <= it - 1 < len(steps):
                                    gens.append(stage3(steps[it - 1])); wts.append(RET_W3)
                                if it < len(steps):
                                    gens.append(stage2(steps[it])); wts.append(RET_W2)
                            else:
                                if it < len(steps):
                                    gens.append(stage2(steps[it])); wts.append(RET_W2)
                                if 0 <= it - 1 < len(steps):
                                    gens.append(stage3(steps[it - 1])); wts.append(RET_W3)
                            interleave(gens, wts)
                        fw.barrier()
            fw.barrier()

        def ffn_block(stack, hT, hT_b, ntok, wg_ap, wu_ap, wd_ap, nf, groups, evac, tag, state=None):
            gmax = max(j1 - j0 for j0, j1 in groups)
            pieces = []
            t0 = 0
            while t0 < ntok:
                n = min(512, ntok - t0)
                pieces.append((t0, n))
                t0 += n
            ntl = ntok // 128
            if state is None:
                state = {}
            if "aT" not in state:
                state["aT"] = sb(stack, "aT" + tag, [128, gmax, ntok], BF16); state["aT_b"] = [Buf() for _ in range(gmax)]
                state["WDq"] = [sb(stack, f"WDq{i}" + tag, [128, gmax, D], BF16) for i in range(2)]; state["WDq_b"] = [Buf(), Buf()]
                state["wgu"] = [sb(stack, f"wgu{i}" + tag, [128, 8, 2, 256], BF16) for i in range(2)]; state["wgu_b"] = [Buf(), Buf()]
                state["sgt"] = [sb(stack, f"sgt{i}" + tag, [128, 512], F32) for i in range(2)]; state["sgt_b"] = [Buf(), Buf()]
                state["PG"] = [ps(stack, f"PG{i}" + tag, [128, 512], F32) for i in range(2)]; state["PG_b"] = [Buf(), Buf()]
                state["PU"] = [ps(stack, f"PU{i}" + tag, [128, 512], F32) for i in range(2)]; state["PU_b"] = [Buf(), Buf()]
                state["PD"] = [ps(stack, f"PD{i}" + tag, [128, 512], F32) for i in range(3)]; state["PD_b"] = [Buf() for _ in range(3)]
                state["d_wg"] = [fw.dsem(f"wgu{i}" + tag) for i in range(2)]
                state["d_wd"] = [fw.dsem(f"wd{i}" + tag) for i in range(2)]
                state["ctr"] = [0, 0, 0, 0]
            aT, aT_b, WDq, WDq_b, wgu, wgu_b = state["aT"], state["aT_b"], state["WDq"], state["WDq_b"], state["wgu"], state["wgu_b"]
            sgt, sgt_b, PG, PG_b, PU, PU_b, PD, PD_b = state["sgt"], state["sgt_b"], state["PG"], state["PG_b"], state["PU"], state["PU_b"], state["PD"], state["PD_b"]
            d_wg, d_wd = state["d_wg"], state["d_wd"]
            wgv, wuv, wdv = wg_ap, wu_ap, wd_ap
            ctr = state["ctr"]
            for gi, (j0, j1) in enumerate(groups):
                ng = j1 - j0
                wb = ctr[3] % 2
                ctr[3] += 1
                fw.dma(pool, WDq[wb][:, 0:ng, :], wdv[:, j0:j1, :], d_wd[wb], writes=[WDq_b[wb]])
                j = j0
                while j < j1:
                    nb = min(2, j1 - j)
                    bi = ctr[0] % 2
                    ctr[0] += 1
                    fw.dma(pool, wgu[bi][:, :, 0, 0:nb * 128], wgv[:, :, j * 128:(j + nb) * 128], d_wg[bi], writes=[wgu_b[bi]])
                    wgu_b[bi].lw = None
                    fw.dma(pool, wgu[bi][:, :, 1, 0:nb * 128], wuv[:, :, j * 128:(j + nb) * 128], d_wg[bi], writes=[wgu_b[bi]])
                    for ch in range(nb):
                        jj = j + ch - j0
                        for (p0, pn) in pieces:
                            pi = ctr[1] % 2
                            ctr[1] += 1
                            for which, (P, P_b) in enumerate(((PG[pi], PG_b[pi]), (PU[pi], PU_b[pi]))):
                                for kc in range(8):
                                    pe.emit(lambda e: e.matmul(P[:, 0:pn], wgu[bi][:, kc, which, ch * 128:(ch + 1) * 128], hT[:, kc, p0:p0 + pn], start=(kc == 0), stop=(kc == 7)),
                                            reads=[wgu_b[bi], hT_b], writes=[P_b], signal=(kc == 7))
                            act.emit(lambda e: e.activation(sgt[pi][:, 0:pn], PG[pi][:, 0:pn], AF.Silu), reads=[PG_b[pi]], writes=[sgt_b[pi]])
                            dve.emit(lambda e: e.tensor_tensor(aT[:, jj, p0:p0 + pn], PU[pi][:, 0:pn], sgt[pi][:, 0:pn], ALU.mult),
                                     reads=[PU_b[pi], sgt_b[pi]], writes=[aT_b[jj]])
                    j += nb
                for tl in range(ntl):
                    for half in range(2):
                        di = ctr[2] % 3
                        ctr[2] += 1
                        for jj in range(ng):
                            pe.emit(lambda e: e.matmul(PD[di][:], aT[:, jj, tl * 128:(tl + 1) * 128], WDq[wb][:, jj, half * 512:(half + 1) * 512], start=(jj == 0), stop=(jj == ng - 1)),
                                    reads=[aT_b[jj], WDq_b[wb]], writes=[PD_b[di]], signal=(jj == ng - 1))
                        evac(tl, half, PD[di], PD_b[di], gi == 0)

        if upto != "ret":
            with ExitStack() as F0:
                MODB = sb(F0, "MODB", [128, 3, D], F32); modb_b = Buf()
                load_mod(0, 1, MODB, modb_b, F0)
                NT0 = 17
                hTa = sb(F0, "hTa", [128, 8, NT0 * 128], BF16); hTa_b = Buf()
                with ExitStack() as T:
                    ssv = [sb(T, f"fss{i}", [128, 16], F32) for i in range(3)]; ss_b = [Buf() for _ in range(3)]
                    hn = [sb(T, f"fhn{i}", [128, D], F32) for i in range(3)]; hn_b = [Buf() for _ in range(3)]
                    h16 = [sb(T, f"fh16{i}", [128, D], BF16) for i in range(2)]; h16_b = [Buf(), Buf()]
                    PTA = [ps(T, f"PTA{i}", [128, 8, 128], BF16) for i in range(2)]; PTA_b = [Buf(), Buf()]

                    def fa(t):
                        i = t % 3
                        act.emit(lambda e: e.activation(hn[i][:], XRES[:, t, :], AF.Square, accum_out=ssv[i][:, 0:1]), reads=[xres_b[t]], writes=[hn_b[i], ss_b[i]])
                        dve.emit(lambda e: e.tensor_scalar(ssv[i][:, 0:1], ssv[i][:, 0:1], 1.0 / D, EPS, ALU.mult, ALU.add), reads=[ss_b[i]], writes=[ss_b[i]])
                        yield

                    def fb(t):
                        i = t % 3
                        k = t % 2
                        act.emit(lambda e: e.activation(ssv[i][:, 0:1], ssv[i][:, 0:1], AF.Sqrt), reads=[ss_b[i]], writes=[ss_b[i]])
                        dve.emit(lambda e: e.reciprocal(ssv[i][:, 0:1], ssv[i][:, 0:1]), reads=[ss_b[i]], writes=[ss_b[i]])
                        dve.emit(lambda e: e.scalar_tensor_tensor(hn[i][:], XRES[:, t, :], ssv[i][:, 0:1], MODB[:, 1, :], ALU.mult, ALU.mult),
                                 reads=[xres_b[t], ss_b[i], modb_b], writes=[hn_b[i]])
                        (pool if t % 2 == 0 else dve).emit(lambda e: e.tensor_tensor(h16[k][:], hn[i][:], MODB[:, 0, :], ALU.add), reads=[hn_b[i], modb_b], writes=[h16_b[k]])
                        yield

                    def fc(t):
                        k = t % 2
                        for kc in range(8):
                            pe.emit(lambda e: e.transpose(PTA[k][:, kc, :], h16[k][:, kc * 128:(kc + 1) * 128], ident[:]),
                                    reads=[h16_b[k], ident_b], writes=[PTA_b[k]], signal=(kc == 7))
                        act.emit(lambda e: e.copy(hTa[:, :, t * 128:(t + 1) * 128], PTA[k][:]), reads=[PTA_b[k]], writes=[hTa_b])
                        yield

                    for it in range(NT0 + 2):
                        gens = []
                        if 0 <= it - 1 < NT0:
                            gens.append(fb(it - 1))
                        if it < NT0:
                            gens.append(fa(it))
                        if 0 <= it - 2 < NT0:
                            gens.append(fc(it - 2))
                        for g_ in gens:
                            for _ in g_:
                                pass
                    fw.barrier()
                with ExitStack() as T:
                    etmp = [sb(T, f"etmp{i}", [128, 512], F32) for i in range(2)]; etmp_b = [Buf(), Buf()]
                    ectr = [0]

                    def evac0(tl, half, bank, bank_b, first):
                        i = ectr[0] % 2
                        ectr[0] += 1
                        xr = XRES[:, tl, half * 512:(half + 1) * 512]
                        dve.emit(lambda e: e.tensor_tensor(etmp[i][:], bank[:], MODB[:, 2, half * 512:(half + 1) * 512], ALU.mult),
                                 reads=[bank_b, modb_b], writes=[etmp_b[i]])
                        (pool if ectr[0] % 3 != 0 else dve).emit(lambda e: e.tensor_tensor(xr, xr, etmp[i][:], ALU.add), reads=[etmp_b[i], xres_b[tl]], writes=[xres_b[tl]])

                    ffn_block(T, hTa, hTa_b, NT0 * 128, ffn_wg[0].rearrange("(k p) n -> p k n", p=128), ffn_wu[0].rearrange("(k p) n -> p k n", p=128),
                              ffn_wd[0].rearrange("(j p) n -> p j n", p=128), NF, [(0, 6), (6, 12), (12, 17), (17, 22)], evac0, "f0")
                    fw.barrier()
                fw.barrier()

        if upto not in ("ret", "ffn"):
            with ExitStack() as P1:
                MODA1 = sb(P1, "MODA1", [128, 3, D], F32); moda1_b = Buf()
                load_mod(1, 0, MODA1, moda1_b, P1)
                WP = sb(P1, "WP", [128, 8, 256], BF16); wp_b = Buf()
                BPB = sb(P1, "BPB", [128, D], F32); bpb_b = Buf()
                SCG = sb(P1, "SCG", [128, D], F32); scg_b = Buf()
                BC = sb(P1, "BC", [128, 4, 128], BF16); bc_b = Buf()
                BP = sb(P1, "BP", [128, 4, 128], BF16); bp_b = Buf()
                BC0 = sb(P1, "BC0", [128, 4, 128], BF16); bc0_b = Buf()
                BP0 = sb(P1, "BP0", [128, 4, 128], BF16); bp0_b = Buf()
                fw.dma(pool, BC[:], pbc_d.rearrange("p (g t) -> p g t", g=4), d_pb[0], writes=[bc_b])
                fw.dma(pool, BP[:], pbp_d.rearrange("p (g t) -> p g t", g=4), d_pb[1], writes=[bp_b])
                fw.dma(pool, BC0[:], pbc0_d.rearrange("p (g t) -> p g t", g=4), d_pb[2], writes=[bc0_b])
                fw.dma(pool, BP0[:], pbp0_d.rearrange("p (g t) -> p g t", g=4), d_pb[3], writes=[bp0_b])
                fw.dma(sp, SCG[:], pool_scale[0, :].partition_broadcast(128), d_const, writes=[scg_b])
                fw.dma(sp, BPB[:], pool_b[0].rearrange("g c -> (g c)").partition_broadcast(128), d_const, writes=[bpb_b])
                dve.emit(lambda e: e.tensor_tensor(SCG[:], SCG[:], MODA1[:, 2, :], ALU.mult), reads=[scg_b, moda1_b], writes=[scg_b])
                dve.emit(lambda e: e.tensor_tensor(BPB[:], BPB[:], SCG[:], ALU.mult), reads=[scg_b, bpb_b], writes=[bpb_b])
                with ExitStack() as st:
                    WPS = sb(st, "WPS", [128, 8, 256], F32); wps_b = Buf()
                    fw.dma(sp, WPS[:], pool_w[0].rearrange("g (k p) d -> p (g k) d", p=128), d_const, writes=[wps_b])
                    for g in range(4):
                        for kc in range(2):
                            dve.emit(lambda e: e.tensor_tensor(WP[:, g * 2 + kc, :], WPS[:, g * 2 + kc, :], SCG[:, g * 256:(g + 1) * 256], ALU.mult),
                                     reads=[wps_b, scg_b], writes=[wp_b])
                    fw.barrier()
                with ExitStack() as T:
                    NH = 3
                    ssv = [sb(T, f"ss{i}", [128, 1], F32) for i in range(2)]; ss_b = [Buf(), Buf()]
                    hn = [sb(T, f"hn{i}", [128, D], F32) for i in range(2)]; hn_b = [Buf(), Buf()]
                    h16 = [sb(T, f"h16{i}", [128, D], BF16) for i in range(NH)]; h16_b = [Buf() for _ in range(NH)]
                    pT16 = [sb(T, f"pT16{i}", [128, 8, 128], BF16) for i in range(2)]; pT16_b = [Buf(), Buf()]
                    PP = [ps(T, f"PP{i}", [128, 8, 128], F32) for i in range(2)]; PP_b = [Buf(), Buf()]
                    PY = [ps(T, f"PY{i}", [128, D], F32) for i in range(2)]; PY_b = [Buf(), Buf()]

                    def pl_a(t):
                        k = t % 2
                        c = t % NH
                        act.emit(lambda e: e.activation(hn[k][:], XRES[:, t, :], AF.Square, accum_out=ssv[k][:]), reads=[xres_b[t]], writes=[hn_b[k], ss_b[k]])
                        dve.emit(lambda e: e.tensor_scalar(ssv[k][:], ssv[k][:], 1.0 / D, EPS, ALU.mult, ALU.add), reads=[ss_b[k]], writes=[ss_b[k]])
                        yield
                        act.emit(lambda e: e.activation(ssv[k][:], ssv[k][:], AF.Sqrt), reads=[ss_b[k]], writes=[ss_b[k]])
                        dve.emit(lambda e: e.reciprocal(ssv[k][:], ssv[k][:]), reads=[ss_b[k]], writes=[ss_b[k]])
                        yield
                        dve.emit(lambda e: e.scalar_tensor_tensor(hn[k][:], XRES[:, t, :], ssv[k][:], MODA1[:, 1, :], ALU.mult, ALU.mult),
                                 reads=[xres_b[t], ss_b[k], moda1_b], writes=[hn_b[k]])
                        (pool if t % 2 == 0 else dve).emit(lambda e: e.tensor_tensor(h16[c][:], hn[k][:], MODA1[:, 0, :], ALU.add), reads=[hn_b[k], moda1_b], writes=[h16_b[c]])
                        yield

                    def pl_b(t):
                        i = t % 2
                        c = t % NH
                        cp = (t - 1) % NH
                        Bcur, Bcur_b = (BC0, bc0_b) if t == 1 else (BC, bc_b)
                        Bprv, Bprv_b = (BP0, bp0_b) if t == 1 else (BP, bp_b)
                        for g in range(4):
                            for kc in range(2):
                                cs_ = slice(g * 256 + kc * 128, g * 256 + (kc + 1) * 128)
                                pe.emit(lambda e: e.matmul(PP[i][:, g * 2 + kc, :], h16[c][:, cs_], Bcur[:, g, :], start=True, stop=False),
                                        reads=[h16_b[c], Bcur_b], writes=[PP_b[i]], signal=False)
                                pe.emit(lambda e: e.matmul(PP[i][:, g * 2 + kc, :], h16[cp][:, cs_], Bprv[:, g, :], start=False, stop=True),
                                        reads=[h16_b[cp], Bprv_b], writes=[PP_b[i]], signal=(g == 3 and kc == 1))
                            if g == 1:
                                yield
                        act.emit(lambda e: e.copy(pT16[i][:], PP[i][:]), reads=[PP_b[i]], writes=[pT16_b[i]])
                        yield
                        for g in range(4):
                            pe.emit(lambda e: e.matmul(PY[i][:, g * 256:(g + 1) * 256], ones16[:], BPR16[:, g * 256:(g + 1) * 256], start=True, stop=False),
                                    reads=[ones16_b, bpr_b], writes=[PY_b[i]], signal=False)
                            for kc in range(2):
                                pe.emit(lambda e: e.matmul(PY[i][:, g * 256:(g + 1) * 256], pT16[i][:, g * 2 + kc, :], WP[:, g * 2 + kc, :], start=False, stop=(kc == 1)),
                                        reads=[pT16_b[i], wp_b], writes=[PY_b[i]], signal=(g == 3 and kc == 1))
                        dve.emit(lambda e: e.tensor_tensor(XRES[:, t, :], PY[i][:], XRES[:, t, :], ALU.add), reads=[PY_b[i], xres_b[t]], writes=[xres_b[t]])
                        yield

                    ones16 = sb(T, "ones16", [1, 128], BF16); ones16_b = Buf()
                    BPR16 = sb(T, "BPR16", [1, D], BF16); bpr_b = Buf()
                    pool.emit(lambda e: e.memset(ones16[:], 1.0), writes=[ones16_b])
                    act.emit(lambda e: e.copy(BPR16[:], BPB[0:1, :]), reads=[bpb_b], writes=[bpr_b])
                    for it in range(17 + 1):
                        gens = []
                        if it < 17:
                            gens.append(pl_a(it))
                        if 1 <= it - 1 <= 16:
                            gens.append(pl_b(it - 1))
                        for g_ in gens:
                            for _ in g_:
                                pass
                    fw.barrier()
                fw.barrier()

        if upto == "all":
            ys_b = [Buf() for _ in range(NSLMAX)]
            d_ys = [fw.dsem(f"ys{i}") for i in range(NSLMAX)]; d_g = [[fw.dsem("g00"), fw.dsem("g01")], [fw.dsem("g10"), fw.dsem("g11")]]
            bc_reg = nc.gpsimd.alloc_register("bcreg")
            nc.gpsimd.reg_mov(bc_reg, TOTS - 1)
            with ExitStack() as M:
                G2V = sb(M, "G2V", [128, D], F32); g2v_b = Buf()
                GATE = sb(M, "GATE", [128, 16, 2], F32); gate_b = Buf()
                POS = sb(M, "POS", [128, 16, 2], mybir.dt.int32); pos_b = [Buf() for _ in range(16)]
                with ExitStack() as R:
                    MODB1 = sb(R, "MODB1", [128, 3, D], F32); modb1_b = Buf()
                    load_mod(1, 1, MODB1, modb1_b, R)
                    dve.emit(lambda e: e.tensor_copy(G2V[:], MODB1[:, 2, :]), reads=[modb1_b], writes=[g2v_b])
                    ident32 = sb(R, "ident32", [128, 128], F32); id32_b = Buf()
                    TRI = sb(R, "TRI", [128, 128], BF16); tri_b = Buf()
                    ONESM = sb(R, "ONESM", [128, 128], BF16); onesm_b = Buf()
                    MT = sb(R, "MT", [128, 96], F32); mt_b = Buf()
                    WR = sb(R, "WR", [128, 8, 8], F32); wr_b = Buf()
                    BASE = sb(R, "BASE", [128, 8], F32); base_b = Buf()
                    pool.emit(lambda e: e.memset(ident32[:], 1.0), writes=[id32_b])
                    pool.emit(lambda e: e.affine_select(ident32[:], ident32[:], [[-1, 128]], ALU.is_equal, 0.0, base=0, channel_multiplier=1), reads=[id32_b], writes=[id32_b])
                    pool.emit(lambda e: e.memset(TRI[:], 1.0), writes=[tri_b])
                    pool.emit(lambda e: e.affine_select(TRI[:], TRI[:], [[1, 128]], ALU.is_gt, 0.0, base=0, channel_multiplier=-1), reads=[tri_b], writes=[tri_b])
                    pool.emit(lambda e: e.memset(ONESM[:], 1.0), writes=[onesm_b])
                    pool.emit(lambda e: e.memset(BASE[:], 0.0), writes=[base_b])
                    fw.dma(sp, MT[:], moetab_d[:, :], d_const, writes=[mt_b])
                    fw.dma(sp, WR[:], moe_router[0].rearrange("(k p) e -> p k e", p=128), d_const, writes=[wr_b])
                    ssv = [sb(R, f"ssr{i}", [128, 1], F32) for i in range(2)]; ss_b = [Buf(), Buf()]
                    hn = [sb(R, f"hnr{i}", [128, D], F32) for i in range(2)]; hn_b = [Buf(), Buf()]
                    h32 = [sb(R, f"h32r{i}", [128, D], F32) for i in range(2)]; h32_b = [Buf(), Buf()]
                    H16 = sb(R, "H16all", [128, 16, D], BF16); h16_b = [Buf() for _ in range(16)]
                    hT32 = [sb(R, f"hT32{i}", [128, 8, 128], F32) for i in range(2)]; hT32_b = [Buf(), Buf()]
                    lg = [sb(R, f"lg{i}", [128, 8], F32) for i in range(2)]; lg_b = [Buf(), Buf()]
                    mx8 = sb(R, "mx8", [128, 8], F32); mx8_b = Buf()
                    A1 = sb(R, "A1all", [128, 16, 8], F32); a1_b = Buf()
                    A2 = sb(R, "A2all", [128, 16, 8], F32); a2_b = Buf()
                    RK = sb(R, "RKall", [128, 16, 8], F32); rk_b = Buf()
                    A16 = sb(R, "A16", [128, 8], BF16); a16_b = Buf()
                    PT32 = [ps(R, f"PT32{i}", [128, 8, 128], F32) for i in range(2)]; pt32_b = [Buf(), Buf()]
                    PLG = [ps(R, f"PLG{i}", [128, 8], F32) for i in range(2)]; plg_b = [Buf(), Buf()]
                    PPR = ps(R, "PPR", [128, 16], F32); ppr_b = Buf()

                    def r_a(ti):
                        t = ti + 1
                        k = ti % 2
                        act.emit(lambda e: e.activation(hn[k][:], XRES[:, t, :], AF.Square, accum_out=ssv[k][:]), reads=[xres_b[t]], writes=[hn_b[k], ss_b[k]])
                        dve.emit(lambda e: e.tensor_scalar(ssv[k][:], ssv[k][:], 1.0 / D, EPS, ALU.mult, ALU.add), reads=[ss_b[k]], writes=[ss_b[k]])
                        yield
                        act.emit(lambda e: e.activation(ssv[k][:], ssv[k][:], AF.Sqrt), reads=[ss_b[k]], writes=[ss_b[k]])
                        dve.emit(lambda e: e.reciprocal(ssv[k][:], ssv[k][:]), reads=[ss_b[k]], writes=[ss_b[k]])
                        yield
                        dve.emit(lambda e: e.scalar_tensor_tensor(hn[k][:], XRES[:, t, :], ssv[k][:], MODB1[:, 1, :], ALU.mult, ALU.mult),
                                 reads=[xres_b[t], ss_b[k], modb1_b], writes=[hn_b[k]])
                        (pool if ti % 2 == 0 else dve).emit(lambda e: e.tensor_tensor(h32[k][:], hn[k][:], MODB1[:, 0, :], ALU.add), reads=[hn_b[k], modb1_b], writes=[h32_b[k]])
                        act.emit(lambda e: e.copy(H16[:, ti, :], h32[k][:]), reads=[h32_b[k]], writes=[h16_b[ti]])
                        yield

                    def r_b(ti):
                        k = ti % 2
                        for kc in range(8):
                            pe.emit(lambda e: e.transpose(PT32[k][:, kc, :], h32[k][:, kc * 128:(kc + 1) * 128], ident32[:]),
                                    reads=[h32_b[k], id32_b], writes=[pt32_b[k]], signal=(kc == 7))
                        dve.emit(lambda e: e.tensor_copy(hT32[k][:], PT32[k][:]), reads=[pt32_b[k]], writes=[hT32_b[k]])
                        yield
                        for kc in range(8):
                            pe.emit(lambda e: e.matmul(PLG[k][:], hT32[k][:, kc, :], WR[:, kc, :], start=(kc == 0), stop=(kc == 7)),
                                    reads=[hT32_b[k], wr_b], writes=[plg_b[k]], signal=(kc == 7))
                        dve.emit(lambda e: e.tensor_copy(lg[k][:], PLG[k][:]), reads=[plg_b[k]], writes=[lg_b[k]])
                        yield

                    M12 = sb(R, "M12", [128, 16, 2], F32); m12_b = Buf()
                    pos8 = [sb(R, f"pos8{i}", [128, 8], F32) for i in range(2)]; pos8_b = [Buf(), Buf()]
                    ovf = [sb(R, f"ovf{i}", [128, 8], F32) for i in range(2)]; ovf_b = [Buf(), Buf()]
                    junk8 = sb(R, "junk8", [128, 8], F32); junk8_b = Buf()
                    posf = [sb(R, f"posf{i}", [128, 16], F32) for i in range(2)]; posf_b = [Buf(), Buf()]

                    def r_c(ti):
                        k = ti % 2
                        i = ti % 2
                        dve.emit(lambda e: e.max(mx8[:], lg[k][:]), reads=[lg_b[k]], writes=[mx8_b])
                        dve.emit(lambda e: e.tensor_scalar(A1[:, ti, :], lg[k][:], mx8[:, 0:1], None, ALU.is_equal), reads=[lg_b[k], mx8_b], writes=[a1_b])
                        dve.emit(lambda e: e.tensor_scalar(A2[:, ti, :], lg[k][:], mx8[:, 1:2], None, ALU.is_equal), reads=[lg_b[k], mx8_b], writes=[a2_b])
                        dve.emit(lambda e: e.tensor_tensor(A16[:], A1[:, ti, :], A2[:, ti, :], ALU.add), reads=[a1_b, a2_b], writes=[a16_b])
                        dve.emit(lambda e: e.tensor_copy(M12[:, ti, :], mx8[:, 0:2]), reads=[mx8_b], writes=[m12_b])
                        yield
                        pe.emit(lambda e: e.matmul(PPR[:, 0:8], TRI[:], A16[:], start=True, stop=True), reads=[tri_b, a16_b], writes=[ppr_b], signal=False)
                        pe.emit(lambda e: e.matmul(PPR[:, 8:16], ONESM[:], A16[:], start=True, stop=True), reads=[onesm_b, a16_b], writes=[ppr_b])
                        yield
                        dve.emit(lambda e: e.tensor_tensor(RK[:, ti, :], PPR[:, 0:8], BASE[:], ALU.add), reads=[ppr_b, base_b], writes=[rk_b])
                        dve.emit(lambda e: e.tensor_tensor(BASE[:], PPR[:, 8:16], BASE[:], ALU.add), reads=[ppr_b, base_b, rk_b], writes=[base_b])
                        dve.emit(lambda e: e.tensor_tensor(ovf[i][:], RK[:, ti, :], MT[:, 0:8], ALU.is_ge), reads=[rk_b, mt_b], writes=[ovf_b[i]])
                        dve.emit(lambda e: e.scalar_tensor_tensor(pos8[i][:], ovf[i][:], 1.0e6, RK[:, ti, :], ALU.mult, ALU.add), reads=[ovf_b[i], rk_b], writes=[pos8_b[i]])
                        dve.emit(lambda e: e.tensor_tensor(pos8[i][:], pos8[i][:], MT[:, 8:16], ALU.add), reads=[pos8_b[i], mt_b], writes=[pos8_b[i]])
                        for kk, (Ak, ak_b) in enumerate(((A1, a1_b), (A2, a2_b))):
                            dve.emit(lambda e: e.scalar_tensor_tensor(junk8[:], Ak[:, ti, :], 1.0, pos8[i][:], ALU.mult, ALU.mult, accum_out=posf[i][:, kk:kk + 1]),
                                     reads=[ak_b, pos8_b[i]], writes=[junk8_b, posf_b[i]])
                        dve.emit(lambda e: e.tensor_copy(POS[:, ti, :], posf[i][:, 0:2]), reads=[posf_b[i]], writes=[pos_b[ti]])
                        yield
                        for kk in range(2):
                            pool.deps([h16_b[ti], pos_b[ti]], [hs_b] if kk == 0 else [])
                            pool.obj.indirect_dma_start(out=HS[:, :], out_offset=bass.IndirectOffsetOnAxis(ap=POS[:, ti, kk:kk + 1], axis=0),
                                                        in_=H16[:, ti, :], in_offset=None, bounds_check=bc_reg, oob_is_err=False).then_inc(d_hs.sem, 16)
                            d_hs.count += 16
                            tok = (d_hs.sem, d_hs.count)
                            fw.note(tok, [h16_b[ti], pos_b[ti]], [])
                        yield

                    for it in range(16 + 2):
                        gens = []
                        if it < 16:
                            gens.append(r_a(it))
                        if 0 <= it - 1 < 16:
                            gens.append(r_b(it - 1))
                        if 0 <= it - 2 < 16:
                            gens.append(r_c(it - 2))
                        for g_ in gens:
                            for _ in g_:
                                pass
                    gd = sb(R, "gd", [128, 16], F32); gd_b = Buf()
                    ge = sb(R, "ge", [128, 16], F32); ge_b = Buf()
                    dve.emit(lambda e: e.tensor_tensor(gd[:], M12[:, :, 1], M12[:, :, 0], ALU.subtract), reads=[m12_b], writes=[gd_b])
                    act.emit(lambda e: e.activation(ge[:], gd[:], AF.Exp), reads=[gd_b], writes=[ge_b])
                    dve.emit(lambda e: e.tensor_scalar(gd[:], ge[:], 1.0, None, ALU.add), reads=[ge_b], writes=[gd_b])
                    dve.emit(lambda e: e.reciprocal(GATE[:, :, 0], gd[:]), reads=[gd_b], writes=[gate_b])
                    dve.emit(lambda e: e.tensor_tensor(GATE[:, :, 1], ge[:], GATE[:, :, 0], ALU.mult), reads=[ge_b, gate_b], writes=[gate_b])
                    hs_b.lw = (d_hs.sem, d_hs.count)
                    hs_b.rd = {}
                    fw.barrier()


                with ExitStack() as E:
                    hs = sb(E, "hs", [128, NSLMAX, D], BF16); hss_b = Buf()
                    hTe = sb(E, "hTe", [128, 8, CAPMAX], BF16); hTe_b = Buf()
                    YACC = sb(E, "YACC", [128, NSLMAX, D], F32); yacc_b = [Buf() for _ in range(NSLMAX)]
                    PTE = ps(E, "PTE", [128, 8, 128], BF16); pte_b = Buf()
                    groups = [(0, 7), (7, 14), (14, 21), (21, 28)]

                    def evacE(tl, half, bank, bank_b, first):
                        ya = YACC[:, tl, half * 512:(half + 1) * 512]
                        if first:
                            act.emit(lambda e: e.copy(ya, bank[:]), reads=[bank_b], writes=[yacc_b[tl]])
                        else:
                            dve.emit(lambda e: e.tensor_tensor(ya, bank[:], ya, ALU.add), reads=[bank_b, yacc_b[tl]], writes=[yacc_b[tl]])

                    with ExitStack() as FB:
                        fb_state = {}
                        for r in range(8):
                            capr = CAPS[r]; nslr = capr // 128; o0 = OFFS[r]
                            fw.dma(sp, hs[:, 0:nslr, :], HS[o0:o0 + capr, :].rearrange("(n p) d -> p n d", p=128), d_x[0], reads=[hs_b], writes=[hss_b])
                            for n in range(nslr):
                                for kc in range(8):
                                    pe.emit(lambda e: e.transpose(PTE[:, kc, :], hs[:, n, kc * 128:(kc + 1) * 128], ident[:]),
                                            reads=[hss_b, ident_b], writes=[pte_b], signal=(kc == 7))
                                act.emit(lambda e: e.copy(hTe[:, :, n * 128:(n + 1) * 128], PTE[:]), reads=[pte_b], writes=[hTe_b])
                            wg_v = moe_wg[0, r].rearrange("(k p) n -> p k n", p=128)
                            wu_v = moe_wu[0, r].rearrange("(k p) n -> p k n", p=128)
                            wd_v = moe_wd[0, r].rearrange("(j p) n -> p j n", p=128)
                            ffn_block(FB, hTe, hTe_b, capr, wg_v, wu_v, wd_v, NFE, groups, evacE, "me", state=fb_state)
                            for tl in range(nslr):
                                fw.dma(act, YS[o0 + tl * 128: o0 + (tl + 1) * 128, :], YACC[:, tl, :], d_ys[tl], reads=[yacc_b[tl]], writes=[ys_b[tl]])
                        fw.barrier()
                    fw.barrier()

                with ExitStack() as C:
                    yg = [[sb(C, f"yg{i}{k}", [128, D], F32) for k in range(2)] for i in range(2)]
                    yg_b = [[Buf(), Buf()], [Buf(), Buf()]]
                    acc = [sb(C, f"acc{i}", [128, D], F32) for i in range(2)]; acc_b = [Buf(), Buf()]
                    FG = sb(C, "FG", [128, D], F32); fg_b = Buf()
                    fw.dma(sp, FG[:], fgain.partition_broadcast(128), d_const, writes=[fg_b])
                    ssv = [sb(C, f"oss{i}", [128, 16], F32) for i in range(2)]; ss_b = [Buf(), Buf()]
                    jk = sb(C, "jk", [128, D], BF16); jk_b = Buf()
                    ho = [sb(C, f"ho{i}", [128, D], F32) for i in range(2)]; ho_b = [Buf(), Buf()]

                    def gath(ti):
                        i = ti % 2
                        for k in range(2):
                            act.emit(lambda e: e.memzero(yg[i][k][:]), writes=[yg_b[i][k]])
                            pool.deps(ys_b + [pos_b[ti]], [yg_b[i][k]])
                            pool.obj.indirect_dma_start(out=yg[i][k][:, :], out_offset=None, in_=YS[:, :],
                                                        in_offset=bass.IndirectOffsetOnAxis(ap=POS[:, ti, k:k + 1], axis=0),
                                                        bounds_check=bc_reg, oob_is_err=False).then_inc(d_g[i][k].sem, 16)
                            d_g[i][k].count += 16
                            tok = (d_g[i][k].sem, d_g[i][k].count)
                            fw.note(tok, [pos_b[ti]], [yg_b[i][k]])

                    def comb(ti):
                        t = ti + 1
                        i = ti % 2
                        act.emit(lambda e: e.activation(acc[i][:], yg[i][0][:], AF.Copy, scale=GATE[:, ti, 0:1]), reads=[yg_b[i][0], gate_b], writes=[acc_b[i]])
                        dve.emit(lambda e: e.scalar_tensor_tensor(acc[i][:], yg[i][1][:], GATE[:, ti, 1:2], acc[i][:], ALU.mult, ALU.add),
                                 reads=[yg_b[i][1], gate_b, acc_b[i]], writes=[acc_b[i]])
                        dve.emit(lambda e: e.tensor_tensor(acc[i][:], acc[i][:], G2V[:], ALU.mult), reads=[acc_b[i], g2v_b], writes=[acc_b[i]])
                        dve.emit(lambda e: e.tensor_tensor(XRES[:, t, :], XRES[:, t, :], acc[i][:], ALU.add), reads=[acc_b[i], xres_b[t]], writes=[xres_b[t]])

                    def fin_a(ti):
                        t = ti + 1
                        i = ti % 2
                        act.emit(lambda e: e.activation(jk[:], XRES[:, t, :], AF.Square, accum_out=ssv[i][:, 0:1]), reads=[xres_b[t]], writes=[jk_b, ss_b[i]])
                        dve.emit(lambda e: e.tensor_scalar(ssv[i][:, 0:1], ssv[i][:, 0:1], 1.0 / D, EPS, ALU.mult, ALU.add), reads=[ss_b[i]], writes=[ss_b[i]])
                        act.emit(lambda e: e.activation(ssv[i][:, 0:1], ssv[i][:, 0:1], AF.Sqrt), reads=[ss_b[i]], writes=[ss_b[i]])
                        dve.emit(lambda e: e.reciprocal(ssv[i][:, 0:1], ssv[i][:, 0:1]), reads=[ss_b[i]], writes=[ss_b[i]])
                        dve.emit(lambda e: e.scalar_tensor_tensor(ho[i][:], XRES[:, t, :], ssv[i][:, 0:1], FG[:], ALU.mult, ALU.mult), reads=[xres_b[t], ss_b[i], fg_b], writes=[ho_b[i]])
                        fw.dma(sp, out_d[ti * 128:(ti + 1) * 128, :], ho[i][:], d_out, reads=[ho_b[i]])

                    gath(0)
                    for it in range(17):
                        if it + 1 < 16:
                            gath(it + 1)
                        if it < 16:
                            comb(it)
                        if it - 1 >= 0:
                            fin_a(it - 1)
                    for d_ in d_out.sems:
                        if d_.count > 0:
                            sp.obj.wait_ge(d_.sem, d_.count)
                    fw.barrier()
                fw.barrier()

        if upto in ("ret", "ffn", "pool"):
            for i in range(16):
                fw.dma(sp, out_d[i * 128:(i + 1) * 128, :], XRES[:, 1 + i, :], d_out, reads=[xres_b[1 + i]])
            for d_ in d_out.sems:
                if d_.count > 0:
                    sp.obj.wait_ge(d_.sem, d_.count)
    return nc


def make_in_maps(inputs):
    x = np.ascontiguousarray(np.asarray(inputs["x"], dtype=np.float32)[0])
    pad = np.zeros(((NDEEP + 1) * 128, D), np.float32)
    xp = np.concatenate([pad, x], 0)
    consts = _const_tables()
    shared = {k: np.ascontiguousarray(np.asarray(v, dtype=np.float32)) for k, v in inputs.items() if k != "x"}
    maps = []
    for c in range(NCORES):
        m = dict(shared)
        m.update(consts)
        m.update(_host_tables(c))
        m["xext"] = np.ascontiguousarray(xp[c * TOWN: c * TOWN + NTILE * 128])
        maps.append(m)
    return maps


def kernel(**inputs):
    nc = build_program("all")
    maps = make_in_maps(inputs)
    res = run_bass_kernel_spmd(nc, maps, core_ids=list(range(NCORES)))
    out = np.concatenate([r["out"] for r in res.results], 0)
    return out.reshape(1, NCORES * TOWN, D).astype(np.float32)
```
